# Optimizing a Trainium2 kernel written in Bass

```python
import jax, jax.numpy as jnp
from jax import lax
import numpy as np

D_MODEL = 1024
BATCH = 8
SEQ = 2048
DEPTH = 4

CHUNK = 64
N_MIXERS = 3
A_HEADS = 16
A_HEAD_DIM = D_MODEL // A_HEADS
A_Q_BLOCK = 128
B_HEADS = 8
B_HEAD_DIM = D_MODEL // B_HEADS
B_CONV = 4
C_WIDTH = 2 * D_MODEL
C_GROUPS = 8
C_SPAN = 128
D_FF = -(-8 * D_MODEL // (3 * 256)) * 256
EPS = 1e-6
N_A = (DEPTH + 2) // 3
N_B = (DEPTH + 1) // 3
N_C = DEPTH // 3

kernel_name = 'fox_mlstm_gmlp_interleaved_trunk'


def rmsnorm(x, g):
    xf = x.astype(jnp.float32)
    y = xf * lax.rsqrt(jnp.mean(xf * xf, axis=-1, keepdims=True) + EPS)
    return (y * g.astype(jnp.float32)).astype(x.dtype)


def forgetting_attention(h, w_in, b_f, w_out):
    B, S, D = h.shape
    proj = h @ w_in
    q, k, v, f = jnp.split(proj, [D, 2 * D, 3 * D], axis=-1)
    to_heads = lambda t: t.reshape(B, S, A_HEADS, A_HEAD_DIM).transpose(0, 2, 1, 3)
    q, k, v = to_heads(q), to_heads(k), to_heads(v)
    logf = jax.nn.log_sigmoid((f + b_f).astype(jnp.float32))
    c = jnp.cumsum(logf, axis=1).transpose(0, 2, 1)
    scale = A_HEAD_DIM ** -0.5
    outs = []
    for j in range(S // A_Q_BLOCK):
        q0, q1 = j * A_Q_BLOCK, (j + 1) * A_Q_BLOCK
        s = jnp.einsum('bhqd,bhkd->bhqk', q[:, :, q0:q1], k[:, :, :q1]).astype(jnp.float32) * scale
        s = s + c[:, :, q0:q1, None] - c[:, :, None, :q1]
        mask = (q0 + jnp.arange(A_Q_BLOCK))[:, None] >= jnp.arange(q1)[None, :]
        p = jax.nn.softmax(jnp.where(mask, s, -jnp.inf), axis=-1)
        outs.append(jnp.einsum('bhqk,bhkd->bhqd', p.astype(v.dtype), v[:, :, :q1]))
    o = jnp.concatenate(outs, axis=2).transpose(0, 2, 1, 3).reshape(B, S, D)
    return o @ w_out


def causal_dwconv(x, w, b):
    K = w.shape[0]
    y = lax.conv_general_dilated(x, w[:, None, :], window_strides=(1,), padding=[(K - 1, 0)],
                                 dimension_numbers=('NWC', 'WIO', 'NWC'),
                                 feature_group_count=x.shape[-1])
    return y + b


def mlstm_mixer(h, w_in, conv_w, conv_b, b_i, b_f, norm_g, w_out):
    B, S, D = h.shape
    H, d, L = B_HEADS, B_HEAD_DIM, CHUNK
    NC = S // L
    proj = h @ w_in
    qk, v, o, ig, fg = jnp.split(proj, [2 * D, 3 * D, 4 * D, 4 * D + H], axis=-1)
    qk = jax.nn.silu(causal_dwconv(qk, conv_w, conv_b))
    q, k = jnp.split(qk, 2, axis=-1)
    to_chunks = lambda t: t.reshape(B, NC, L, H, d).transpose(1, 0, 3, 2, 4).astype(jnp.float32)
    q, k, v = to_chunks(q), to_chunks(k) * (d ** -0.5), to_chunks(v)
    gate_chunks = lambda t: t.astype(jnp.float32).reshape(B, NC, L, H).transpose(1, 0, 3, 2)
    ig = gate_chunks(ig + b_i)
    lf = gate_chunks(jax.nn.log_sigmoid((fg + b_f).astype(jnp.float32)))
    tri = jnp.arange(L)[:, None] >= jnp.arange(L)[None, :]

    def step(carry, inp):
        C, n, m = carry
        qc, kc, vc, igc, lfc = inp
        bcum = jnp.cumsum(lfc, axis=-1)
        log_D = jnp.where(tri, bcum[..., :, None] - bcum[..., None, :] + igc[..., None, :], -jnp.inf)
        m_inter = bcum + m[..., None]
        m_t = jnp.maximum(m_inter, jnp.max(log_D, axis=-1))
        Dm = jnp.exp(log_D - m_t[..., None])
        a = jnp.exp(m_inter - m_t)
        sqk = jnp.einsum('bhtd,bhsd->bhts', qc, kc) * Dm
        num = a[..., None] * jnp.einsum('bhtd,bhde->bhte', qc, C) + jnp.einsum('bhts,bhse->bhte', sqk, vc)
        den = a * jnp.einsum('bhtd,bhd->bht', qc, n) + jnp.sum(sqk, axis=-1)
        hc = num / jnp.maximum(jnp.abs(den), jnp.exp(-m_t))[..., None]
        bL = bcum[..., -1]
        g = bL[..., None] - bcum + igc
        m_new = jnp.maximum(bL + m, jnp.max(g, axis=-1))
        w = jnp.exp(g - m_new[..., None])
        decay = jnp.exp(bL + m - m_new)
        C = decay[..., None, None] * C + jnp.einsum('bhsd,bhse->bhde', kc * w[..., None], vc)
        n = decay[..., None] * n + jnp.einsum('bhs,bhsd->bhd', w, kc)
        return (C, n, m_new), hc

    init = (jnp.zeros((B, H, d, d), jnp.float32), jnp.zeros((B, H, d), jnp.float32),
            jnp.zeros((B, H), jnp.float32))
    _, hs = lax.scan(step, init, (q, k, v, ig, lf))
    hs = hs.transpose(1, 0, 3, 2, 4).reshape(B, S, H, d)
    hs = hs * lax.rsqrt(jnp.mean(hs * hs, axis=-1, keepdims=True) + EPS)
    hs = hs.reshape(B, S, D) * norm_g.astype(jnp.float32)
    out = hs.astype(h.dtype) * jax.nn.sigmoid(o)
    return out @ w_out


def gmlp_mixer(h, w_in, ln_g, ln_b, w_s, b_s, w_out):
    B, S, D = h.shape
    z = jax.nn.gelu(h @ w_in, approximate=False)
    u, v = jnp.split(z, 2, axis=-1)
    vf = v.astype(jnp.float32)
    mu = jnp.mean(vf, axis=-1, keepdims=True)
    var = jnp.mean(jnp.square(vf - mu), axis=-1, keepdims=True)
    v = ((vf - mu) * lax.rsqrt(var + EPS) * ln_g + ln_b).astype(h.dtype)
    v = v.reshape(B, S // C_SPAN, C_SPAN, C_GROUPS, C_WIDTH // C_GROUPS)
    pos = jnp.arange(C_SPAN)
    mask = (pos[:, None] // CHUNK) >= (pos[None, :] // CHUNK)
    ws = w_s * mask
    s = jnp.einsum('gts,bnsgc->bntgc', ws, v) + b_s.T[None, None, :, :, None]
    out = u * s.reshape(B, S, C_WIDTH)
    return out @ w_out


def swiglu(h, w_gu, w_down):
    g, u = jnp.split(h @ w_gu, 2, axis=-1)
    return (jax.nn.silu(g) * u) @ w_down


def setup_inputs(seed: int = 0) -> dict:
    key = jax.random.key(seed)
    ks = iter(jax.random.split(key, 32))
    nrm = lambda shape, scale: jax.random.normal(next(ks), shape, jnp.float32) * scale
    D = D_MODEL
    return {
        'x': nrm((BATCH, SEQ, D), 1.0),
        'norm1_g': 1.0 + nrm((DEPTH, D), 0.02),
        'norm2_g': 1.0 + nrm((DEPTH, D), 0.02),
        'final_g': 1.0 + nrm((D,), 0.02),
        'a_w_in': nrm((N_A, D, 3 * D + A_HEADS), D ** -0.5),
        'a_b_f': jnp.linspace(1.0, 5.0, A_HEADS)[None, :] + nrm((N_A, A_HEADS), 0.1),
        'a_w_out': nrm((N_A, D, D), D ** -0.5),
        'b_w_in': nrm((N_B, D, 4 * D + 2 * B_HEADS), D ** -0.5),
        'b_conv_w': nrm((N_B, B_CONV, 2 * D), B_CONV ** -0.5),
        'b_conv_b': nrm((N_B, 2 * D), 0.01),
        'b_b_i': nrm((N_B, B_HEADS), 0.1),
        'b_b_f': jnp.linspace(3.0, 6.0, B_HEADS)[None, :] + nrm((N_B, B_HEADS), 0.1),
        'b_norm_g': 1.0 + nrm((N_B, D), 0.02),
        'b_w_out': nrm((N_B, D, D), D ** -0.5),
        'c_w_in': nrm((N_C, D, 2 * C_WIDTH), D ** -0.5),
        'c_ln_g': 1.0 + nrm((N_C, C_WIDTH), 0.02),
        'c_ln_b': nrm((N_C, C_WIDTH), 0.01),
        'c_w_s': nrm((N_C, C_GROUPS, C_SPAN, C_SPAN), C_SPAN ** -0.5),
        'c_b_s': 1.0 + nrm((N_C, C_GROUPS, C_SPAN), 0.02),
        'c_w_out': nrm((N_C, C_WIDTH, D), C_WIDTH ** -0.5),
        'ffn_w_gu': nrm((DEPTH, D, 2 * D_FF), D ** -0.5),
        'ffn_w_down': nrm((DEPTH, D_FF, D), D_FF ** -0.5),
    }


def reference(x, norm1_g, norm2_g, final_g, a_w_in, a_b_f, a_w_out, b_w_in, b_conv_w, b_conv_b,
              b_b_i, b_b_f, b_norm_g, b_w_out, c_w_in, c_ln_g, c_ln_b, c_w_s, c_b_s, c_w_out,
              ffn_w_gu, ffn_w_down):
    for i in range(DEPTH):
        kind, j = i % N_MIXERS, i // N_MIXERS
        hn = rmsnorm(x, norm1_g[i])
        if kind == 0:
            y = forgetting_attention(hn, a_w_in[j], a_b_f[j], a_w_out[j])
        elif kind == 1:
            y = mlstm_mixer(hn, b_w_in[j], b_conv_w[j], b_conv_b[j], b_b_i[j], b_b_f[j],
                            b_norm_g[j], b_w_out[j])
        else:
            y = gmlp_mixer(hn, c_w_in[j], c_ln_g[j], c_ln_b[j], c_w_s[j], c_b_s[j], c_w_out[j])
        x = x + y
        x = x + swiglu(rmsnorm(x, norm2_g[i]), ffn_w_gu[i], ffn_w_down[i])
    return rmsnorm(x, final_g)
```

```python
import contextlib
import numpy as np
import concourse.bass as bass
import concourse.mybir as mybir
from concourse.bass_utils import run_bass_kernel_spmd

F32 = mybir.dt.float32
BF16 = mybir.dt.bfloat16
AF = mybir.ActivationFunctionType
ALU = mybir.AluOpType

T = 2048
D = 1024
KC = 8
NTG = 4
DFF = 2816
NF = 22
EPS = 1e-6
DEPTH = 4
SLOT = 4096
NSLOT = 3
ACTT_NF = NF


class Sched:
    def __init__(self, nc):
        self.nc = nc
        self.eng = {"pe": nc.tensor, "act": nc.scalar, "dve": nc.vector, "pool": nc.gpsimd,
                    "sp": nc.sync}
        self.sem = {}
        self.cnt = {}
        self.waited = {}
        self.last_w = {}
        self.reads = {}
        self.pending = {e: False for e in self.eng}
        self.nsem = 0

    def _sem(self, key):
        if key not in self.sem:
            self.sem[key] = self.nc.alloc_semaphore("s_" + str(key))
            self.cnt[key] = 0
            self.nsem += 1
        return self.sem[key]

    def _wait(self, e, deps):
        best = {}
        for (k, v) in deps:
            if v > best.get(k, 0):
                best[k] = v
        for k, v in best.items():
            if k == "pe" and e == "pe":
                continue
            if self.waited.get((e, k), 0) >= v:
                continue
            assert self.cnt[k] >= v, ("dependency on unsignaled instruction", e, k, v, self.cnt[k])
            self.eng[e].wait_ge(self.sem[k], v)
            self.waited[(e, k)] = v

    def _deps(self, r, w):
        deps = []
        for b in r:
            if b in self.last_w:
                deps.append(self.last_w[b])
        for b in w:
            if b in self.last_w:
                deps.append(self.last_w[b])
            deps.extend(self.reads.get(b, ()))
        return deps

    def _record(self, r, w, tag):
        for b in r:
            self.reads.setdefault(b, []).append(tag)
        for b in w:
            self.last_w[b] = tag
            self.reads[b] = []

    def op(self, e, fn, r=(), w=(), sig=True):
        self._sem(e)
        self._wait(e, self._deps(r, w))
        ins = fn(self.eng[e])
        if sig:
            self.cnt[e] += 1
            ins.then_inc(self.sem[e], 1)
            tag = (e, self.cnt[e])
            self.pending[e] = False
        else:
            tag = (e, self.cnt[e] + 1)
            self.pending[e] = True
        self._record(r, w, tag)
        return ins

    def dma(self, q, out, in_, r=(), w=(), chan=None, **kw):
        assert chan is not None
        self._sem(chan)
        self._wait(q, self._deps(r, w))
        ins = self.eng[q].dma_start(out=out, in_=in_, **kw)
        self.cnt[chan] += 16
        ins.then_inc(self.sem[chan], 16)
        self._record(r, w, (chan, self.cnt[chan]))
        return ins

    def barrier(self):
        for e in self.eng:
            assert not self.pending[e]
        keys = [k for k in self.sem if self.cnt[k] > 0]
        for e in self.eng:
            for k in keys:
                if k == e:
                    continue
                if self.waited.get((e, k), 0) >= self.cnt[k]:
                    continue
                self.eng[e].wait_ge(self.sem[k], self.cnt[k])
                self.waited[(e, k)] = self.cnt[k]

    def finish(self, chans):
        for c in chans:
            if c in self.sem and self.cnt[c] > 0:
                self.eng["sp"].wait_ge(self.sem[c], self.cnt[c])


class WPipe:
    def __init__(self, S, slots, depth):
        self.S = S
        self.slots = slots
        self.depth = depth
        self.plan = []
        self.issued = 0
        self.taken = 0
        self.dry = True

    def _issue(self):
        i = self.issued
        ap, n = self.plan[i]
        s = i % len(self.slots)
        self.S.dma("pool", self.slots[s][:, 0:n], ap, w=[("wslot", s)], chan=("wch", s),
                   max_dma_last_dim=2048 * 4)
        self.issued += 1

    def next(self, ap, n):
        if self.dry:
            self.plan.append((ap, n))
            return self.slots[0], ("wslot", 0)
        i = self.taken
        self.taken += 1
        while self.issued < min(len(self.plan), i + self.depth):
            self._issue()
        s = i % len(self.slots)
        return self.slots[s], ("wslot", s)


def blockify_cols(W, cw):
    K, N = W.shape
    kc = K // 128
    nb = N // cw
    a = W.reshape(kc, 128, nb, cw).transpose(2, 1, 0, 3)
    return np.ascontiguousarray(a).reshape(nb, 128, kc * cw)


def col_layout(v):
    return np.ascontiguousarray(v.reshape(-1, 128).T)


class Ctx:
    pass


ARENA_ELEMS = 38 * 1024


class Arena:
    def __init__(self, C):
        self.C = C
        self.off = 0

    def bf(self, n):
        a = self.C.ARENA[:, self.off:self.off + n]
        self.off += n + (n % 2)
        assert self.off <= ARENA_ELEMS, self.off
        return a

    def f32(self, n):
        return self.bf(2 * n).bitcast(F32)


def build(stages, dbg=False):
    nc = bass.Bass("TRN2", target_bir_lowering=False)
    S = Sched(nc)
    C = Ctx()
    C.nc, C.S = nc, S
    es = contextlib.ExitStack()

    def dram(name, shape, kind="ExternalInput", dt=F32):
        return nc.dram_tensor(name, list(shape), dt, kind=kind).ap()

    C.xT = dram("xT", [D, T])
    C.outT = dram("outT", [D, T], kind="ExternalOutput")
    C.vecs = dram("vecs", [128, 80])
    C.ffn_gu = dram("ffn_gu", [DEPTH, NF, 128, KC * 256])
    C.ffn_dn = dram("ffn_dn", [DEPTH, KC, 128, NF * 128])
    declare_mixer_inputs(C, dram)

    with es:
        sb = lambda name, shape, dt: es.enter_context(nc.sbuf_tensor(name, list(shape), dt))
        C.sb = sb
        C.XT = sb("XT", [128, KC, T], F32)
        C.VEC = sb("VEC", [128, 80], F32)
        C.ones_bf = sb("ones_bf", [128, 128], BF16)
        C.epsc = sb("epsc", [128, 1], F32)
        C.slots = [sb("wslot%d" % i, [128, SLOT], BF16) for i in range(NSLOT)]
        C.ARENA = sb("ARENA", [128, ARENA_ELEMS], BF16)
        alloc_mixer_consts(C)
        C.PB = [es.enter_context(nc.psum_tensor("pb%d" % i, [128, 1024], F32)) for i in range(4)]
        C.pbi = 0
        C.W = WPipe(S, C.slots, NSLOT)

        def emit_all():
            prologue(C)
            for st in stages:
                st(C)
            epilogue(C)

        C.W.dry = True
        C.dry = True
        real = (S.op, S.dma, S.barrier)
        S.op = lambda *a, **k: None
        S.dma = lambda *a, **k: None
        S.barrier = lambda *a, **k: None
        emit_all()
        S.op, S.dma, S.barrier = real
        C.W.dry = False
        C.dry = False
        C.pbi = 0
        emit_all()
        S.finish([("och",)])
    return nc


def pbank(C):
    i = C.pbi % 8
    C.pbi += 1
    return C.PB[i // 2][:, (i % 2) * 512:(i % 2 + 1) * 512], ("bk", i)


def pbank2(C):
    if C.pbi % 2:
        C.pbi += 1
    i = C.pbi % 8
    C.pbi += 2
    return C.PB[i // 2][:, :], [("bk", i), ("bk", i + 1)]


def prologue(C):
    S = C.S
    xv = C.xT.rearrange("(c p) t -> p c t", p=128)
    for n in range(NTG):
        S.dma("sp", C.XT[:, :, n * 512:(n + 1) * 512], xv[:, :, n * 512:(n + 1) * 512],
              w=[("XT", c, n) for c in range(KC)], chan=("xch", n))
    S.dma("sp", C.VEC[:], C.vecs[:], w=["VEC"], chan="vch")
    S.op("dve", lambda e: e.memset(C.ones_bf[:], 1.0), w=["ones_bf"])
    S.op("dve", lambda e: e.memset(C.epsc[:], EPS), w=["epsc"])
    mixer_prologue(C)


def epilogue(C):
    S = C.S
    S.barrier()
    A = Arena(C)
    tmp = norm_tmp(A)
    ov = C.outT.rearrange("(c p) t -> p c t", p=128)
    for n in range(NTG):
        rmsnorm(C, 64, lambda c, n: (C.XT[:, c, n * 512:(n + 1) * 512], ("XT", c, n)), [n], tmp)
        S.dma("sp", ov[:, :, n * 512:(n + 1) * 512], C.XT[:, :, n * 512:(n + 1) * 512],
              r=[("XT", c, n) for c in range(KC)], chan=("och",))


def norm_tmp(A):
    return ([A.bf(512) for _ in range(2)], [A.f32(512) for _ in range(2)])


def rmsnorm(C, gcol, out_fn, groups, tmp):
    S = C.S
    sqs, rss = tmp
    for n in groups:
        ts = slice(n * 512, (n + 1) * 512)
        pb, pk = pbank(C)
        for c in range(KC):
            sq = sqs[c % 2]
            S.op("act", lambda e: e.activation(out=sq, in_=C.XT[:, c, ts], func=AF.Square),
                 r=[("XT", c, n)], w=[("sq", c % 2)])
            S.op("pe", lambda e: e.matmul(pb, C.ones_bf[:], sq, start=(c == 0),
                                          stop=(c == KC - 1)),
                 r=["ones_bf", ("sq", c % 2)], w=[pk], sig=True)
        rs = rss[n % 2]
        rk = ("rs", n % 2)
        S.op("act", lambda e: e.activation(out=rs, in_=pb, func=AF.Sqrt,
                                           scale=1.0 / D, bias=C.epsc[:, 0:1]),
             r=[pk, "epsc"], w=[rk])
        S.op("dve", lambda e: e.reciprocal(out=rs, in_=rs), r=[rk], w=[rk])
        for c in range(KC):
            oap, okey = out_fn(c, n)
            S.op("dve", lambda e: e.scalar_tensor_tensor(
                out=oap, in0=C.XT[:, c, ts], scalar=C.VEC[:, gcol + c:gcol + c + 1], in1=rs,
                op0=ALU.mult, op1=ALU.mult),
                r=[("XT", c, n), "VEC", rk], w=[okey])


def swiglu(C, layer):
    S = C.S
    S.barrier()
    A = Arena(C)
    tmp = norm_tmp(A)
    HTg = A.bf(KC * 1024).rearrange("p (k t) -> p k t", k=KC)
    ACTT = A.bf(NF * 1024).rearrange("p (f t) -> p f t", f=NF)
    sgs = [A.f32(1024) for _ in range(2)]
    for half in range(2):
        t0 = half * 1024
        rmsnorm(C, 32 + 8 * layer,
                lambda c, n: (HTg[:, c, (n % 2) * 512:(n % 2 + 1) * 512], ("HTg", c, n % 2)),
                [2 * half, 2 * half + 1], tmp)
        for f in range(NF):
            wt, wk = C.W.next(C.ffn_gu[layer, f], KC * 256)
            wv = wt[:, 0:KC * 256].rearrange("p (k c) -> p k c", k=KC)
            pg, pgk = pbank2(C)
            pu, puk = pbank2(C)
            for (pp, ppk, off) in ((pg, pgk, 0), (pu, puk, 128)):
                for n2 in range(2):
                    for kc in range(KC):
                        S.op("pe", lambda e: e.matmul(
                            pp[:, n2 * 512:(n2 + 1) * 512], wv[:, kc, off:off + 128],
                            HTg[:, kc, n2 * 512:(n2 + 1) * 512], start=(kc == 0),
                            stop=(kc == KC - 1)),
                            r=[wk, ("HTg", kc, n2)], w=[ppk[n2]], sig=(kc == KC - 1))
            sg = sgs[f % 2]
            sgk = ("sg", f % 2)
            S.op("act", lambda e: e.activation(out=sg, in_=pg[:, :], func=AF.Silu),
                 r=pgk, w=[sgk])
            S.op("dve", lambda e: e.tensor_tensor(out=ACTT[:, f, :], in0=pu[:, :], in1=sg,
                                                  op=ALU.mult),
                 r=puk + [sgk], w=[("ACTT", f)])
        for m in range(KC):
            wt, wk = C.W.next(C.ffn_dn[layer, m], NF * 128)
            wv = wt[:, 0:NF * 128].rearrange("p (f c) -> p f c", f=NF)
            py, pyk = pbank2(C)
            for n2 in range(2):
                for f in range(NF):
                    S.op("pe", lambda e: e.matmul(
                        py[:, n2 * 512:(n2 + 1) * 512], wv[:, f, :],
                        ACTT[:, f, n2 * 512:(n2 + 1) * 512], start=(f == 0), stop=(f == NF - 1)),
                        r=[wk, ("ACTT", f)], w=[pyk[n2]], sig=(f == NF - 1))
            S.op("dve", lambda e: e.tensor_tensor(
                out=C.XT[:, m, t0:t0 + 1024], in0=py[:, :], in1=C.XT[:, m, t0:t0 + 1024],
                op=ALU.add),
                r=pyk + [("XT", m, half * 2), ("XT", m, half * 2 + 1)],
                w=[("XT", m, half * 2), ("XT", m, half * 2 + 1)])


def ffn_stage(layer):
    def st(C):
        swiglu(C, layer)
    return st


C_W = 2048
MV_LNG = 0
MV_ABF = 16
MV_CW = 32
MV_CB = 96
MV_BI = 112
MV_BF = 113
MV_NCOL = 128


def declare_mixer_inputs(C, dram):
    C.mvec = dram("mvec", [128, MV_NCOL])
    C.c_win = dram("c_win", [8, 128, KC * 512])
    C.c_wout = dram("c_wout", [KC, 128, 16 * 128])
    C.c_wsT = dram("c_wsT", [128, 1024])
    C.c_lnb = dram("c_lnb", [1, C_W])
    C.c_bs = dram("c_bs", [1, 1024])
    C.consts = dram("consts", [128, 5 * 128])
    C.a_win = dram("a_win", [2, 8, 128, KC * 384])
    C.a_wf = dram("a_wf", [2, 128, KC * 16])
    C.a_wout = dram("a_wout", [2, 8, 128, D])
    C.b_win = dram("b_win", [8, 128, KC * 512])
    C.b_wg = dram("b_wg", [128, KC * 16])
    C.b_wout = dram("b_wout", [8, 128, D])
    C.b_ng = dram("b_ng", [128, D])


def alloc_mixer_consts(C):
    C.MVEC = C.sb("MVEC", [128, MV_NCOL], F32)
    C.CONST = C.sb("CONST", [128, 5 * 128], F32)
    C.ident_bf = C.sb("ident_bf", [128, 128], BF16)
    C.negmask_bf = C.sb("negmask_bf", [128, 128], BF16)


def mixer_prologue(C):
    C.S.dma("sp", C.MVEC[:], C.mvec[:], w=["MVEC"], chan="vch")
    C.S.dma("sp", C.CONST[:], C.consts[:], w=["CONST"], chan="vch")
    C.S.dma("pool", C.ident_bf[:], C.consts[:, 0:128], w=["ident_bf"], chan="cch")
    C.S.dma("pool", C.negmask_bf[:], C.consts[:, 256:384], w=["negmask_bf"], chan="cch")
    C.ident = C.CONST[:, 0:128]
    C.sel63 = C.CONST[:, 128:256]
    C.negmask = C.CONST[:, 256:384]
    C.sel127 = C.CONST[:, 384:512]
    C.tri01 = C.CONST[:, 512:640]


def gmlp(C, layer):
    S = C.S
    S.barrier()
    A = Arena(C)
    tmp = norm_tmp(A)
    HTg = A.bf(KC * 512).rearrange("p (k t) -> p k t", k=KC)
    Vtok = A.bf(4 * C_W).rearrange("p (n f) -> p n f", n=4)
    UT = A.bf(16 * 512).rearrange("p (f t) -> p f t", f=16)
    junk = A.bf(512)
    tmps = [A.f32(512) for _ in range(2)]
    E = A.f32(16 * 128).rearrange("p (f t) -> p f t", f=16)
    wsT = A.bf(1024)
    LB2 = A.f32(C_W)
    R2 = A.f32(1024)
    S1 = A.f32(16)
    S2 = A.f32(16)
    st4 = A.f32(16)

    S.dma("pool", wsT, C.c_wsT[:], w=["wsT"], chan="cch")
    S.op("dve", lambda e: e.memset(wsT[64:128, :].rearrange("p (g t) -> p g t", g=8)[:, :, 0:64],
                                   0.0), r=[], w=["wsT"])
    S.op("dve", lambda e: e.memset(LB2[0:2, :], 1.0), w=["LB2"])
    S.dma("sp", LB2[0:1, :], C.c_lnb[:], w=["LB2"], chan="cch2")
    S.dma("sp", R2[1:2, :], C.c_bs[:], w=["R2b"], chan="cch3")
    for i in range(2):
        pb, pk = pbank(C)
        S.op("pe", lambda e: e.matmul(pb[0:1, :], C.ones_bf[:, 0:1],
                                      wsT[:, i * 512:(i + 1) * 512], start=True, stop=True),
             r=["wsT", "ones_bf"], w=[pk])
        S.op("act", lambda e: e.activation(out=R2[0:1, i * 512:(i + 1) * 512], in_=pb[0:1, :],
                                           func=AF.Copy), r=[pk], w=["R2a%d" % i])
    for fc in range(16):
        g = fc // 2
        if fc % 4 == 0:
            pb, pk = pbank(C)
        S.op("pe", lambda e: e.matmul(pb[:, (fc % 4) * 128:(fc % 4 + 1) * 128],
                                      LB2[0:2, fc * 128:(fc + 1) * 128],
                                      R2[0:2, g * 128:(g + 1) * 128], start=True, stop=True),
             r=["LB2", "R2b", "R2a0", "R2a1"], w=[pk])
        if fc % 4 == 3:
            f0 = fc - 3
            S.op("act", lambda e: e.activation(
                out=E[:, f0:f0 + 4, :], in_=pb.rearrange("p (f t) -> p f t", f=4),
                func=AF.Copy), r=[pk], w=["E"])

    for tg in range(NTG):
        rmsnorm(C, 8 * layer, lambda c, n: (HTg[:, c, :], ("HTg", c)), [tg], tmp)
        for j in range(4):
            wt, wk = C.W.next(C.c_win[j], KC * 512)
            wv = wt[:, 0:KC * 512].rearrange("p (k c) -> p k c", k=KC)
            for q in range(4):
                fc = j * 4 + q
                po, pk = pbank(C)
                for kc in range(KC):
                    S.op("pe", lambda e: e.matmul(po, wv[:, kc, q * 128:(q + 1) * 128],
                                                  HTg[:, kc, :], start=(kc == 0),
                                                  stop=(kc == KC - 1)),
                         r=[wk, ("HTg", kc)], w=[pk], sig=(kc == KC - 1))
                S.op("act", lambda e: e.activation(out=UT[:, fc, :], in_=po, func=AF.Gelu),
                     r=[pk], w=[("UT", fc)])
        for j in range(4):
            wt, wk = C.W.next(C.c_win[4 + j], KC * 512)
            wv = wt[:, 0:KC * 512].rearrange("p (k c) -> p k c", k=KC)
            for n in range(4):
                po, pk = pbank(C)
                for kc in range(KC):
                    S.op("pe", lambda e: e.matmul(po, HTg[:, kc, n * 128:(n + 1) * 128],
                                                  wv[:, kc, :], start=(kc == 0),
                                                  stop=(kc == KC - 1)),
                         r=[wk, ("HTg", kc)], w=[pk], sig=(kc == KC - 1))
                vb = Vtok[:, n, j * 512:(j + 1) * 512]
                col = n * 4 + j
                S.op("act", lambda e: e.activation(out=vb, in_=po, func=AF.Gelu,
                                                   accum_out=S1[:, col:col + 1]),
                     r=[pk], w=[("Vtok", n, j), ("S1", col)])
                S.op("act", lambda e: e.activation(out=junk, in_=vb, func=AF.Square,
                                                   accum_out=S2[:, col:col + 1]),
                     r=[("Vtok", n, j)], w=["junk", ("S2", col)])
        allS1 = [("S1", c) for c in range(16)]
        allS2 = [("S2", c) for c in range(16)]
        S.op("dve", lambda e: e.tensor_reduce(out=st4[:, 0:4],
                                              in_=S1.rearrange("p (n j) -> p n j", n=4),
                                              op=ALU.add, axis=mybir.AxisListType.X),
             r=allS1, w=["st_mu"])
        S.op("dve", lambda e: e.tensor_reduce(out=st4[:, 4:8],
                                              in_=S2.rearrange("p (n j) -> p n j", n=4),
                                              op=ALU.add, axis=mybir.AxisListType.X),
             r=allS2, w=["st_ex2"])
        S.op("dve", lambda e: e.tensor_scalar(out=st4[:, 0:8], in0=st4[:, 0:8], scalar1=1.0 / C_W,
                                              scalar2=None, op0=ALU.mult),
             r=["st_mu", "st_ex2"], w=["st_mu", "st_ex2"])
        S.op("dve", lambda e: e.tensor_tensor(out=st4[:, 8:12], in0=st4[:, 0:4], in1=st4[:, 0:4],
                                              op=ALU.mult), r=["st_mu"], w=["st_var"])
        S.op("dve", lambda e: e.tensor_tensor(out=st4[:, 8:12], in0=st4[:, 4:8], in1=st4[:, 8:12],
                                              op=ALU.subtract), r=["st_ex2", "st_var"], w=["st_var"])
        S.op("act", lambda e: e.activation(out=st4[:, 8:12], in_=st4[:, 8:12], func=AF.Sqrt,
                                           bias=C.epsc[:, 0:1]), r=["st_var", "epsc"], w=["st_var"])
        S.op("dve", lambda e: e.reciprocal(out=st4[:, 12:16], in_=st4[:, 8:12]),
             r=["st_var"], w=["st_rstd"])
        for n in range(4):
            S.op("dve", lambda e: e.tensor_scalar(
                out=Vtok[:, n, :], in0=Vtok[:, n, :], scalar1=st4[:, n:n + 1],
                scalar2=st4[:, 12 + n:13 + n], op0=ALU.subtract, op1=ALU.mult),
                r=["st_mu", "st_rstd"] + [("Vtok", n, j) for j in range(4)],
                w=[("Vtok", n, j) for j in range(4)])
        for fc in range(16):
            g = fc // 2
            pb, pk = pbank(C)
            for n in range(4):
                S.op("pe", lambda e: e.matmul(pb[:, n * 128:(n + 1) * 128],
                                              Vtok[:, n, fc * 128:(fc + 1) * 128],
                                              wsT[:, g * 128:(g + 1) * 128], start=True, stop=True),
                     r=["wsT"] + [("Vtok", n, j) for j in range(4)], w=[pk], sig=(n == 3))
            tm = tmps[fc % 2]
            tk = ("tmp", fc % 2)
            S.op("dve", lambda e: e.scalar_tensor_tensor(
                out=tm.rearrange("p (n t) -> p n t", n=4),
                in0=pb.rearrange("p (n t) -> p n t", n=4),
                scalar=C.MVEC[:, MV_LNG + fc:MV_LNG + fc + 1],
                in1=E[:, fc:fc + 1, :].to_broadcast([128, 4, 128]),
                op0=ALU.mult, op1=ALU.add), r=[pk, "MVEC", "E"], w=[tk])
            S.op("dve", lambda e: e.tensor_tensor(out=UT[:, fc, :], in0=tm, in1=UT[:, fc, :],
                                                  op=ALU.mult), r=[tk, ("UT", fc)], w=[("UT", fc)])
        for m in range(KC):
            wt, wk = C.W.next(C.c_wout[m], 16 * 128)
            wv = wt[:, 0:16 * 128].rearrange("p (f c) -> p f c", f=16)
            po, pk = pbank(C)
            for fc in range(16):
                S.op("pe", lambda e: e.matmul(po, wv[:, fc, :], UT[:, fc, :], start=(fc == 0),
                                              stop=(fc == 15)),
                     r=[wk, ("UT", fc)], w=[pk], sig=(fc == 15))
            S.op("dve", lambda e: e.tensor_tensor(
                out=C.XT[:, m, tg * 512:(tg + 1) * 512], in0=po,
                in1=C.XT[:, m, tg * 512:(tg + 1) * 512], op=ALU.add),
                r=[pk, ("XT", m, tg)], w=[("XT", m, tg)])


def bank(C, i):
    return C.PB[i // 2][:, (i % 2) * 512:(i % 2 + 1) * 512], ("bk", i)


def fox(C, layer):
    S = C.S
    j_ = layer // 3
    S.barrier()
    A = Arena(C)
    HT = A.bf(KC * T).rearrange("p (k t) -> p k t", k=KC)
    a_tok = A.f32(256)
    amid = A.f32(256)
    negb = A.f32(2)
    QTz = [A.bf(T) for _ in range(2)]
    KTa = [A.bf(T) for _ in range(2)]
    Vtm = A.bf(16 * 128).rearrange("p (n e) -> p n e", n=16)
    augrow = [QTz[0][64:65, :], QTz[1][0:1, :]]
    mark = A.off
    tmp = norm_tmp(A)
    A.off = mark
    sp = A.f32(T)
    acs = A.f32(T)
    onesrow = A.bf(T)
    A.off = mark
    PTs = [A.bf(512) for _ in range(3)]
    OT = A.bf(T)
    rec = [A.f32(512) for _ in range(2)]

    rmsnorm(C, 8 * layer, lambda c, n: (HT[:, c, n * 512:(n + 1) * 512], ("HT", c, n)),
            range(NTG), tmp)
    S.barrier()

    wt, wk = C.W.next(C.a_wf[j_], KC * 16)
    wv = wt[:, 0:KC * 16].rearrange("p (k c) -> p k c", k=KC)
    S.op("dve", lambda e: e.tensor_scalar(out=negb[0:16, 0:1],
                                          in0=C.MVEC[0:16, MV_ABF + j_:MV_ABF + j_ + 1],
                                          scalar1=-1.0, scalar2=None, op0=ALU.mult),
         r=["MVEC"], w=["negb"])
    S.op("dve", lambda e: e.memset(onesrow[0:16, :], 1.0), w=["onesrow"])
    S.op("dve", lambda e: e.memset(negb[0:16, 1:2], 1.0), w=["one1"])
    for n in range(NTG):
        pb, pk = pbank(C)
        for kc in range(KC):
            S.op("pe", lambda e: e.matmul(pb[0:16, :], wv[:, kc, :], HT[:, kc, n * 512:(n + 1) * 512],
                                          start=(kc == 0), stop=(kc == KC - 1)),
                 r=[wk, ("HT", kc, n)], w=[pk], sig=(kc == KC - 1))
        S.op("act", lambda e: e.activation(out=sp[0:16, n * 512:(n + 1) * 512], in_=pb[0:16, :],
                                           func=AF.Exp, scale=-1.0, bias=negb[0:16, 0:1]),
             r=[pk, "negb"], w=[("sp", n)])
    S.op("act", lambda e: e.activation(out=sp[0:16, :], in_=sp[0:16, :], func=AF.Ln,
                                       bias=negb[0:16, 1:2]),
         r=[("sp", n) for n in range(NTG)] + ["one1"], w=["spl"])
    S.op("dve", lambda e: e.tensor_tensor_scan(out=acs[0:16, :], data0=onesrow[0:16, :],
                                               data1=sp[0:16, :], initial=0.0,
                                               op0=ALU.mult, op1=ALU.add),
         r=["spl", "onesrow"], w=["acs"])
    pb, pk = pbank(C)
    for n in range(16):
        S.op("pe", lambda e: e.transpose(pb[:, n * 16:(n + 1) * 16], acs[0:16, n * 128:(n + 1) * 128],
                                         C.ident[0:16, 0:16]),
             r=["acs", "CONST"], w=[pk], sig=(n == 15))
    S.op("dve", lambda e: e.tensor_copy(out=a_tok, in_=pb[:, 0:256]), r=[pk], w=["a_tok"])
    pb, pk = pbank(C)
    S.op("pe", lambda e: e.matmul(pb[:, 0:256], C.sel63, a_tok, start=True, stop=True),
         r=["a_tok", "CONST"], w=[pk])
    S.op("dve", lambda e: e.tensor_copy(out=amid, in_=pb[:, 0:256]), r=[pk], w=["amid"])
    S.barrier()

    m3 = amid.rearrange("p (t h) -> p h t", h=16)
    scale = 64 ** -0.5
    BK_ST, BK_OT, BK_DEN, BK_MISC = (0, 1), (2, 3), (4, 5), (6, 7)
    misc_i = [0]

    def misc_bank():
        b = BK_MISC[misc_i[0] % 2]
        misc_i[0] += 1
        return bank(C, b)

    S.op("dve", lambda e: e.memset(QTz[0][64:128, :], 0.0), w=[("QTpad", 0)])
    S.op("dve", lambda e: e.memset(QTz[1][0:64, :], 0.0), w=[("QTpad", 1)])
    S.op("dve", lambda e: e.memset(KTa[0][64:128, :], 0.0), w=[("KTpad", 0)])
    S.op("dve", lambda e: e.memset(KTa[1][0:64, :], 0.0), w=[("KTpad", 1)])
    S.op("dve", lambda e: e.memset(KTa[0][64:65, :], 1.0), r=[("KTpad", 0)], w=[("KTpad", 0)])
    S.op("dve", lambda e: e.memset(KTa[1][0:1, :], 1.0), r=[("KTpad", 1)], w=[("KTpad", 1)])

    for hp in range(8):
        wt, wk = C.W.next(C.a_win[j_, hp], KC * 384)
        wv = wt[:, 0:KC * 384].rearrange("p (k c) -> p k c", k=KC)
        for (dst, off, key) in ((QTz, 0, "QT"), (KTa, 128, "KT")):
            for n in range(NTG):
                pb, pk = misc_bank()
                for kc in range(KC):
                    S.op("pe", lambda e: e.matmul(pb, wv[:, kc, off:off + 128],
                                                  HT[:, kc, n * 512:(n + 1) * 512],
                                                  start=(kc == 0), stop=(kc == KC - 1)),
                         r=[wk, ("HT", kc, n)], w=[pk], sig=(kc == KC - 1))
                for hh in range(2):
                    p0 = hh * 64
                    S.op("dve", lambda e: e.tensor_copy(
                        out=dst[hh][p0:p0 + 64, n * 512:(n + 1) * 512], in_=pb[p0:p0 + 64, :]),
                        r=[pk], w=[(key, hh, n)])
        for n4 in range(4):
            pb, pk = misc_bank()
            for i in range(4):
                n = n4 * 4 + i
                for kc in range(KC):
                    S.op("pe", lambda e: e.matmul(pb[:, i * 128:(i + 1) * 128],
                                                  HT[:, kc, n * 128:(n + 1) * 128],
                                                  wv[:, kc, 256:384],
                                                  start=(kc == 0), stop=(kc == KC - 1)),
                         r=[wk, ("HT", kc, n // 4)], w=[pk], sig=(kc == KC - 1 and i == 3))
            S.op("act", lambda e: e.activation(
                out=Vtm[:, n4 * 4:n4 * 4 + 4, :],
                in_=pb.rearrange("p (n e) -> p n e", n=4), func=AF.Copy),
                r=[pk], w=[("V", n4)])
        for hh in range(2):
            h = hp * 2 + hh
            pr = 64 if hh == 0 else 0
            S.op("dve", lambda e: e.tensor_scalar(
                out=augrow[hh].rearrange("p (j t) -> p j t", j=16),
                in0=m3[pr:pr + 1, h, :].rearrange("p (j o) -> p j o", o=1)
                .to_broadcast([1, 16, 128]),
                scalar1=-1.0 / scale, scalar2=None, op0=ALU.mult),
                r=["amid", ("QTpad", hh)], w=[("aug", hh)])

        steps = [(hh, Q, kb) for hh in range(2) for Q in range(4) for kb in range(4 * Q + 4)]

        def emit_scores(idx):
            hh, Q, kb = steps[idx]
            h = hp * 2 + hh
            i = max(0, kb - 4 * Q)
            q0 = Q * 512 + i * 128
            N = 512 - i * 128
            diag = kb >= 4 * Q
            st, stk = bank(C, BK_ST[idx % 2])
            S.op("pe", lambda e: e.matmul(st[:, 0:N], KTa[hh][:, kb * 128:(kb + 1) * 128],
                                          QTz[hh][:, q0:q0 + N], start=True, stop=not diag),
                 r=[("KT", hh, kb // 4), ("KTpad", hh), ("QT", hh, Q), ("QTpad", hh), ("aug", hh)],
                 w=[stk], sig=not diag)
            if diag:
                S.op("pe", lambda e: e.matmul(st[:, 0:128], C.ident_bf[:], C.negmask_bf[:],
                                              start=False, stop=True),
                     r=["ident_bf", "negmask_bf"], w=[stk])
            P = PTs[idx % 3]
            S.op("act", lambda e: e.activation(out=P[:, 0:N], in_=st[:, 0:N], func=AF.Exp,
                                               scale=scale,
                                               bias=a_tok[:, kb * 16 + h:kb * 16 + h + 1]),
                 r=[stk, "a_tok"], w=[("PT", idx % 3)])

        def emit_pv(idx):
            hh, Q, kb = steps[idx]
            p0 = hh * 64
            i = max(0, kb - 4 * Q)
            N = 512 - i * 128
            g = hh * 4 + Q
            ot, otk = bank(C, BK_OT[g % 2])
            dn, dnk = bank(C, BK_DEN[g % 2])
            P = PTs[idx % 3]
            last = (kb == 4 * Q + 3)
            S.op("pe", lambda e: e.matmul(ot[p0:p0 + 64, i * 128:512], Vtm[:, kb, p0:p0 + 64],
                                          P[:, 0:N], start=(kb == 0), stop=last),
                 r=[("V", kb // 4), ("PT", idx % 3)], w=[otk], sig=False)
            S.op("pe", lambda e: e.matmul(dn[p0:p0 + 64, i * 128:512], C.ones_bf[:, 0:64],
                                          P[:, 0:N], start=(kb == 0), stop=last),
                 r=["ones_bf", ("PT", idx % 3)], w=[dnk])
            if last:
                rc = rec[g % 2]
                S.op("dve", lambda e: e.reciprocal(out=rc[p0:p0 + 64, :], in_=dn[p0:p0 + 64, :]),
                     r=[dnk], w=[("rec", g % 2)])
                S.op("dve", lambda e: e.tensor_tensor(
                    out=OT[p0:p0 + 64, Q * 512:(Q + 1) * 512], in0=ot[p0:p0 + 64, :],
                    in1=rc[p0:p0 + 64, :], op=ALU.mult),
                    r=[otk, ("rec", g % 2)], w=[("OT", Q)])

        for idx in range(len(steps)):
            emit_scores(idx)
            if idx >= 1:
                emit_pv(idx - 1)
        emit_pv(len(steps) - 1)

        wt, wk = C.W.next(C.a_wout[j_, hp], D)
        for m in range(KC):
            for n in range(NTG):
                pb, pk = misc_bank()
                S.op("pe", lambda e: e.matmul(pb, wt[:, m * 128:(m + 1) * 128],
                                              OT[:, n * 512:(n + 1) * 512], start=True, stop=True),
                     r=[wk, ("OT", n)], w=[pk])
                S.op("dve", lambda e: e.tensor_tensor(
                    out=C.XT[:, m, n * 512:(n + 1) * 512], in0=pb,
                    in1=C.XT[:, m, n * 512:(n + 1) * 512], op=ALU.add),
                    r=[pk, ("XT", m, n)], w=[("XT", m, n)])


def mlstm(C, layer):
    S = C.S
    S.barrier()
    A = Arena(C)
    HT = A.bf(KC * T).rearrange("p (k t) -> p k t", k=KC)
    GTok = A.f32(384).rearrange("p (a t h) -> p a t h", a=3, t=16)
    Mend = A.f32(128)
    wkk = A.f32(128)
    uu = A.f32(128)
    dec = A.f32(128)
    enm = A.f32(128)
    NGh = A.f32(128)
    gsm = A.f32(8)
    mark = A.off
    tmp = norm_tmp(A)
    rmsnorm(C, 8 * layer, lambda c, n: (HT[:, c, n * 512:(n + 1) * 512], ("HT", c, n)),
            range(NTG), tmp)
    S.barrier()
    A.off = mark
    T1 = A.f32(T)
    T2 = A.f32(T)
    T3 = A.f32(T)
    onesrow = A.bf(T)

    wt, wk = C.W.next(C.b_wg[:], KC * 16)
    wv = wt[:, 0:KC * 16].rearrange("p (k c) -> p k c", k=KC)
    S.op("dve", lambda e: e.tensor_scalar(out=gsm[0:8, 0:1], in0=C.MVEC[0:8, MV_BF:MV_BF + 1],
                                          scalar1=-1.0, scalar2=None, op0=ALU.mult),
         r=["MVEC"], w=["gsm0"])
    S.op("dve", lambda e: e.memset(gsm[0:8, 1:2], 1.0), w=["gsm1"])
    S.op("dve", lambda e: e.memset(onesrow[0:8, :], 1.0), w=["onesrow"])
    igp = []
    for n in range(NTG):
        pi, pik = pbank(C)
        pf, pfk = pbank(C)
        for (pp, ppk, off) in ((pi, pik, 0), (pf, pfk, 8)):
            for kc in range(KC):
                S.op("pe", lambda e: e.matmul(pp[0:8, :], wv[:, kc, off:off + 8],
                                              HT[:, kc, n * 512:(n + 1) * 512],
                                              start=(kc == 0), stop=(kc == KC - 1)),
                     r=[wk, ("HT", kc, n)], w=[ppk], sig=(kc == KC - 1))
        S.op("act", lambda e: e.activation(out=T1[0:8, n * 512:(n + 1) * 512], in_=pf[0:8, :],
                                           func=AF.Exp, scale=-1.0, bias=gsm[0:8, 0:1]),
             r=[pfk, "gsm0"], w=[("T1", n)])
        S.op("dve", lambda e: e.tensor_scalar(out=T3[0:8, n * 512:(n + 1) * 512], in0=pi[0:8, :],
                                              scalar1=C.MVEC[0:8, MV_BI:MV_BI + 1], scalar2=None,
                                              op0=ALU.add), r=[pik, "MVEC"], w=[("T3", n)])
    allT = lambda nm: [(nm, n) for n in range(NTG)]
    S.op("act", lambda e: e.activation(out=T1[0:8, :], in_=T1[0:8, :], func=AF.Ln,
                                       bias=gsm[0:8, 1:2]), r=allT("T1") + ["gsm1"], w=allT("T1"))
    S.op("dve", lambda e: e.tensor_tensor_scan(out=T2[0:8, :], data0=onesrow[0:8, :],
                                               data1=T1[0:8, :], initial=0.0,
                                               op0=ALU.mult, op1=ALU.add),
         r=allT("T1") + ["onesrow"], w=["T2"])
    S.op("dve", lambda e: e.tensor_tensor(out=T1[0:8, :], in0=T3[0:8, :], in1=T2[0:8, :],
                                          op=ALU.add), r=allT("T3") + ["T2"], w=allT("T1"))
    S.op("dve", lambda e: e.tensor_tensor_scan(out=T3[0:8, :], data0=T1[0:8, :], data1=T1[0:8, :],
                                               initial=0.0, op0=ALU.max, op1=ALU.max),
         r=allT("T1"), w=allT("T3"))
    S.op("dve", lambda e: e.tensor_tensor(out=T2[0:8, :], in0=T2[0:8, :], in1=T3[0:8, :],
                                          op=ALU.subtract), r=["T2"] + allT("T3"), w=["T2"])
    pb, pk = pbank(C)
    srcs = (T1, T3, T2)
    for a_ in range(3):
        for n in range(16):
            col = (a_ * 16 + n) * 8
            S.op("pe", lambda e: e.transpose(pb[:, col:col + 8],
                                             srcs[a_][0:8, n * 128:(n + 1) * 128],
                                             C.ident[0:8, 0:8]),
                 r=allT("T1") + allT("T3") + ["T2", "CONST"], w=[pk], sig=(a_ == 2 and n == 15))
    S.op("dve", lambda e: e.tensor_copy(out=GTok.rearrange("p a t h -> p (a t h)"),
                                        in_=pb[:, 0:384]), r=[pk], w=["GTok"])
    Rt = GTok[:, 0].rearrange("p t h -> p (t h)")
    Mt = GTok[:, 1].rearrange("p t h -> p (t h)")
    NMt = GTok[:, 2].rearrange("p t h -> p (t h)")
    pb, pk = pbank(C)
    S.op("pe", lambda e: e.matmul(pb[:, 0:128], C.sel127, Mt, start=True, stop=True),
         r=["GTok", "CONST"], w=[pk])
    S.op("dve", lambda e: e.tensor_copy(out=Mend, in_=pb[:, 0:128]), r=[pk], w=["Mend"])
    kscale = 128 ** -0.5
    S.op("dve", lambda e: e.tensor_tensor(out=wkk, in0=Rt, in1=Mend, op=ALU.subtract),
         r=["GTok", "Mend"], w=["wkk"])
    S.op("act", lambda e: e.activation(out=wkk, in_=wkk, func=AF.Exp), r=["wkk"], w=["wkk"])
    S.op("dve", lambda e: e.tensor_scalar(out=wkk, in0=wkk, scalar1=kscale, scalar2=None,
                                          op0=ALU.mult), r=["wkk"], w=["wkk"])
    S.op("dve", lambda e: e.tensor_tensor(out=uu, in0=Mend, in1=Mt, op=ALU.subtract),
         r=["GTok", "Mend"], w=["uu"])
    S.op("act", lambda e: e.activation(out=uu, in_=uu, func=AF.Exp), r=["uu"], w=["uu"])
    S.op("act", lambda e: e.activation(out=enm, in_=NMt, func=AF.Exp), r=["GTok"], w=["enm"])
    S.op("dve", lambda e: e.tensor_scalar(out=dec[:, 0:8], in0=Mend[:, 0:8], scalar1=-1.0,
                                          scalar2=None, op0=ALU.mult), r=["Mend"], w=["dec0"])
    S.op("dve", lambda e: e.tensor_tensor(out=dec[:, 8:128], in0=Mend[:, 0:120],
                                          in1=Mend[:, 8:128], op=ALU.subtract),
         r=["Mend"], w=["dec1"])
    S.op("act", lambda e: e.activation(out=dec, in_=dec, func=AF.Exp), r=["dec0", "dec1"],
         w=["dec"])
    S.barrier()
    A.off = mark
    QT = A.bf(T)
    KT = A.bf(T)
    Ktok = A.bf(16 * 128).rearrange("p (n f) -> p n f", n=16)
    Vaug = A.bf(16 * 130).rearrange("p (n e) -> p n e", n=16)
    OG = A.bf(16 * 128).rearrange("p (n e) -> p n e", n=16)
    Htok = KT.rearrange("p (n e) -> p n e", n=16)
    HR = A.f32(16 * 130).rearrange("p (n e) -> p n e", n=16)
    ep = A.f32(6 * 16).rearrange("p (a c) -> p a c", a=6)
    uu3 = uu.rearrange("p (c h) -> p c h", h=8)
    enm3 = enm.rearrange("p (c h) -> p c h", h=8)
    XPs = [A.f32(516) for _ in range(2)]
    cts = [A.f32(512) for _ in range(2)]
    Pm = [A.bf(128) for _ in range(2)]
    Vt = [A.bf(130) for _ in range(2)]
    Sst = [A.f32(130) for _ in range(2)]
    Sbf = [A.bf(130) for _ in range(2)]
    junk = A.bf(128)
    S.op("dve", lambda e: e.memset(Vaug[:, :, 128:129], 1.0), w=["Vones"])

    for h in range(8):
        wt, wk = C.W.next(C.b_win[h], KC * 512)
        wv = wt[:, 0:KC * 512].rearrange("p (k c) -> p k c", k=KC)
        S.dma("sp", NGh, C.b_ng[:, h * 128:(h + 1) * 128], w=["NG"], chan="cch2")
        for (dst, off, ch, key) in ((QT, 0, h, "QT"), (KT, 128, 8 + h, "KT")):
            S.op("dve", lambda e: e.memset(XPs[0][:, 0:3], 0.0), r=[], w=[("XPh", 0)])
            cw = lambda j: C.MVEC[:, MV_CW + j * 16 + ch:MV_CW + j * 16 + ch + 1]
            for n in range(NTG):
                XPg, ctmp = XPs[n % 2], cts[n % 2]
                xk, xh, ck = ("XPg", n % 2), ("XPh", n % 2), ("ctmp", n % 2)
                pb, pk = pbank(C)
                for kc in range(KC):
                    S.op("pe", lambda e: e.matmul(pb, wv[:, kc, off:off + 128],
                                                  HT[:, kc, n * 512:(n + 1) * 512],
                                                  start=(kc == 0), stop=(kc == KC - 1)),
                         r=[wk, ("HT", kc, n)], w=[pk], sig=(kc == KC - 1))
                S.op("act", lambda e: e.activation(out=XPg[:, 3:515], in_=pb, func=AF.Copy),
                     r=[pk], w=[xk])
                S.op("act", lambda e: e.activation(
                    out=ctmp, in_=pb, func=AF.Identity, scale=cw(3),
                    bias=C.MVEC[:, MV_CB + ch:MV_CB + ch + 1]),
                    r=[pk, "MVEC"], w=[ck])
                if n < NTG - 1:
                    S.op("act", lambda e: e.activation(out=XPs[(n + 1) % 2][:, 0:3],
                                                       in_=XPg[:, 512:515], func=AF.Copy),
                         r=[xk], w=[("XPh", (n + 1) % 2)])
                for j in (2, 1, 0):
                    S.op("dve", lambda e: e.scalar_tensor_tensor(
                        out=ctmp, in0=XPg[:, j:j + 512], scalar=cw(j), in1=ctmp,
                        op0=ALU.mult, op1=ALU.add), r=[xk, xh, "MVEC", ck], w=[ck])
                S.op("act", lambda e: e.activation(out=dst[:, n * 512:(n + 1) * 512], in_=ctmp,
                                                   func=AF.Silu), r=[ck], w=[(key, n)])
        for n4 in range(4):
            pb, pk = pbank(C)
            pbb = pb.bitcast(BF16)
            for i in range(4):
                n = n4 * 4 + i
                S.op("pe", lambda e: e.transpose(pbb[:, i * 128:(i + 1) * 128],
                                                 KT[:, n * 128:(n + 1) * 128], C.ident_bf[:]),
                     r=[("KT", n4), "ident_bf"], w=[pk], sig=(i == 3))
            S.op("act", lambda e: e.activation(
                out=Ktok[:, n4 * 4:n4 * 4 + 4, :],
                in_=pbb[:, 0:512].rearrange("p (n f) -> p n f", n=4), func=AF.Copy),
                r=[pk], w=[("Ktok", n4)])
        for n4 in range(4):
            pv, pvk = pbank(C)
            po_, pok_ = pbank(C)
            for (pp, ppk, off) in ((pv, pvk, 256), (po_, pok_, 384)):
                for i in range(4):
                    n = n4 * 4 + i
                    for kc in range(KC):
                        S.op("pe", lambda e: e.matmul(pp[:, i * 128:(i + 1) * 128],
                                                      HT[:, kc, n * 128:(n + 1) * 128],
                                                      wv[:, kc, off:off + 128],
                                                      start=(kc == 0), stop=(kc == KC - 1)),
                             r=[wk, ("HT", kc, n4)], w=[ppk], sig=(kc == KC - 1 and i == 3))
            S.op("act", lambda e: e.activation(
                out=Vaug[:, n4 * 4:n4 * 4 + 4, 0:128],
                in_=pv.rearrange("p (n e) -> p n e", n=4), func=AF.Copy),
                r=[pvk, "Vones"], w=[("V", n4)])
            S.op("act", lambda e: e.activation(
                out=OG[:, n4 * 4:n4 * 4 + 4, :], in_=po_.rearrange("p (n e) -> p n e", n=4),
                func=AF.Sigmoid), r=[pok_], w=[("OG", n4)])
            S.op("dve", lambda e: e.tensor_tensor(
                out=OG[:, n4 * 4:n4 * 4 + 4, :], in0=OG[:, n4 * 4:n4 * 4 + 4, :],
                in1=NGh.rearrange("p (o e) -> p o e", o=1).to_broadcast([128, 4, 128]),
                op=ALU.mult),
                r=[("OG", n4), "NG"], w=[("OG", n4)])
        for c in range(16):
            col = c * 8 + h
            cs = slice(c * 128, (c + 1) * 128)
            ps_, psk = pbank(C)
            S.op("pe", lambda e: e.matmul(ps_[:, 0:128], KT[:, cs], QT[:, cs], start=True,
                                          stop=True), r=[("KT", c // 4), ("QT", c // 4)], w=[psk])
            P = Pm[c % 2]
            S.op("dve", lambda e: e.scalar_tensor_tensor(
                out=P, in0=ps_[:, 0:128], scalar=wkk[:, col:col + 1], in1=C.tri01,
                op0=ALU.mult, op1=ALU.mult), r=[psk, "wkk", "CONST"], w=[("P", c % 2)])
            V_ = Vt[c % 2]
            S.op("dve", lambda e: e.tensor_scalar(out=V_[:, 0:129], in0=Vaug[:, c, 0:129],
                                                  scalar1=wkk[:, col:col + 1], scalar2=None,
                                                  op0=ALU.mult),
                 r=[("V", c // 4), "Vones", "wkk"], w=[("Vt", c % 2)])
            pu_, puk_ = pbank(C)
            S.op("pe", lambda e: e.matmul(pu_[:, 0:129], Ktok[:, c, :], V_[:, 0:129], start=True,
                                          stop=True), r=[("Ktok", c // 4), ("Vt", c % 2)],
                 w=[puk_])
            Sc, Sn = Sst[c % 2], Sst[(c + 1) % 2]
            if c == 0:
                S.op("dve", lambda e: e.tensor_copy(out=Sn[:, 0:129], in_=pu_[:, 0:129]),
                     r=[puk_], w=[("Sst", (c + 1) % 2)])
            else:
                S.op("dve", lambda e: e.scalar_tensor_tensor(
                    out=Sn[:, 0:129], in0=Sc[:, 0:129], scalar=dec[:, col:col + 1],
                    in1=pu_[:, 0:129], op0=ALU.mult, op1=ALU.add),
                    r=[puk_, ("Sst", c % 2), "dec"], w=[("Sst", (c + 1) % 2)])
            po, pok = pbank(C)
            if c > 0:
                Sb = Sbf[c % 2]
                S.op("act", lambda e: e.activation(out=Sb[:, 0:129], in_=Sc[:, 0:129],
                                                   func=AF.Copy, scale=dec[:, col:col + 1]),
                     r=[("Sst", c % 2), "dec"], w=[("Sbf", c % 2)])
                S.op("pe", lambda e: e.matmul(po[:, 0:129], QT[:, cs], Sb[:, 0:129], start=True,
                                              stop=False), r=[("QT", c // 4), ("Sbf", c % 2)],
                     w=[pok], sig=False)
            S.op("pe", lambda e: e.matmul(po[:, 0:129], P, Vaug[:, c, 0:129], start=(c == 0),
                                          stop=True),
                 r=[("P", c % 2), ("V", c // 4), "Vones"], w=[pok])
            S.op("act", lambda e: e.activation(out=HR[:, c, 0:129], in_=po[:, 0:129],
                                               func=AF.Copy), r=[pok], w=[("HR", c)])
        allHR = [("HR", c) for c in range(16)]
        den = HR[:, :, 128]
        S.op("dve", lambda e: e.tensor_tensor(out=ep[:, 0, :], in0=den, in1=uu3[:, :, h],
                                              op=ALU.mult), r=allHR + ["uu"], w=["ep0"])
        S.op("dve", lambda e: e.tensor_scalar(out=ep[:, 1, :], in0=ep[:, 0, :], scalar1=-1.0,
                                              scalar2=None, op0=ALU.mult), r=["ep0"], w=["ep1"])
        S.op("dve", lambda e: e.tensor_tensor(out=ep[:, 0, :], in0=ep[:, 0, :], in1=ep[:, 1, :],
                                              op=ALU.max), r=["ep0", "ep1"], w=["ep0"])
        S.op("dve", lambda e: e.tensor_tensor(out=ep[:, 0, :], in0=ep[:, 0, :], in1=enm3[:, :, h],
                                              op=ALU.max), r=["ep0", "enm"], w=["ep0"])
        S.op("dve", lambda e: e.reciprocal(out=ep[:, 0, :], in_=ep[:, 0, :]), r=["ep0"], w=["ep0"])
        S.op("dve", lambda e: e.tensor_tensor(out=ep[:, 2, :], in0=ep[:, 0, :], in1=uu3[:, :, h],
                                              op=ALU.mult), r=["ep0", "uu"], w=["ep2"])
        for c in range(16):
            S.op("act", lambda e: e.activation(out=junk, in_=HR[:, c, 0:128], func=AF.Square,
                                               accum_out=ep[:, 3, c:c + 1]),
                 r=[("HR", c)], w=["junk", ("ss", c)])
        S.op("dve", lambda e: e.tensor_tensor(out=ep[:, 4, :], in0=ep[:, 2, :], in1=ep[:, 2, :],
                                              op=ALU.mult), r=["ep2"], w=["ep4"])
        S.op("dve", lambda e: e.tensor_tensor(out=ep[:, 4, :], in0=ep[:, 4, :], in1=ep[:, 3, :],
                                              op=ALU.mult), r=["ep4"] + [("ss", c) for c in range(16)],
             w=["ep4"])
        S.op("act", lambda e: e.activation(out=ep[:, 4, :], in_=ep[:, 4, :], func=AF.Sqrt,
                                           scale=1.0 / 128, bias=C.epsc[:, 0:1]),
             r=["ep4", "epsc"], w=["ep4"])
        S.op("dve", lambda e: e.reciprocal(out=ep[:, 4, :], in_=ep[:, 4, :]), r=["ep4"], w=["ep4"])
        S.op("dve", lambda e: e.tensor_tensor(out=ep[:, 5, :], in0=ep[:, 4, :], in1=ep[:, 2, :],
                                              op=ALU.mult), r=["ep4", "ep2"], w=["ep5"])
        for c in range(16):
            S.op("dve", lambda e: e.scalar_tensor_tensor(
                out=Htok[:, c, :], in0=HR[:, c, 0:128], scalar=ep[:, 5, c:c + 1], in1=OG[:, c, :],
                op0=ALU.mult, op1=ALU.mult),
                r=[("HR", c), "ep5", ("OG", c // 4), ("KT", c // 4)], w=[("KT", c // 4)])
        for n4 in range(4):
            pb, pk = pbank(C)
            pbb = pb.bitcast(BF16)
            for i in range(4):
                n = n4 * 4 + i
                S.op("pe", lambda e: e.transpose(pbb[:, i * 128:(i + 1) * 128], Htok[:, n, :],
                                                 C.ident_bf[:]),
                     r=[("KT", n4), "ident_bf"], w=[pk], sig=(i == 3))
            S.op("act", lambda e: e.activation(out=QT[:, n4 * 512:(n4 + 1) * 512],
                                               in_=pbb[:, 0:512], func=AF.Copy),
                 r=[pk], w=[("QT", n4)])
        wt, wk = C.W.next(C.b_wout[h], D)
        for m in range(KC):
            for n in range(NTG):
                pb, pk = pbank(C)
                S.op("pe", lambda e: e.matmul(pb, wt[:, m * 128:(m + 1) * 128],
                                              QT[:, n * 512:(n + 1) * 512], start=True, stop=True),
                     r=[wk, ("QT", n)], w=[pk])
                S.op("dve", lambda e: e.tensor_tensor(
                    out=C.XT[:, m, n * 512:(n + 1) * 512], in0=pb,
                    in1=C.XT[:, m, n * 512:(n + 1) * 512], op=ALU.add),
                    r=[pk, ("XT", m, n)], w=[("XT", m, n)])


def mixer_stage(i):
    kind = i % 3
    def st(C):
        if kind == 2:
            gmlp(C, i)
        elif kind == 0:
            fox(C, i)
        else:
            mlstm(C, i)
    return st


def prep_shared(inp):
    sh = {}
    vecs = np.zeros((128, 80), np.float32)
    for i in range(DEPTH):
        vecs[:, 8 * i:8 * i + 8] = col_layout(inp["norm1_g"][i])
        vecs[:, 32 + 8 * i:32 + 8 * i + 8] = col_layout(inp["norm2_g"][i])
    vecs[:, 64:72] = col_layout(inp["final_g"])
    sh["vecs"] = vecs
    gu = np.empty((DEPTH, NF, 128, KC * 256), np.float32)
    dn = np.empty((DEPTH, KC, 128, NF * 128), np.float32)
    for i in range(DEPTH):
        W = inp["ffn_w_gu"][i]
        g = W[:, :DFF].reshape(KC, 128, NF, 128)
        u = W[:, DFF:].reshape(KC, 128, NF, 128)
        cat = np.concatenate([g, u], axis=3)
        gu[i] = cat.transpose(2, 1, 0, 3).reshape(NF, 128, KC * 256)
        Wd = inp["ffn_w_down"][i].reshape(NF, 128, KC, 128)
        dn[i] = Wd.transpose(2, 1, 0, 3).reshape(KC, 128, NF * 128)
    sh["ffn_gu"] = gu
    sh["ffn_dn"] = dn
    mvec = np.zeros((128, MV_NCOL), np.float32)
    mvec[:, MV_LNG:MV_LNG + 16] = col_layout(inp["c_ln_g"][0])
    for j in range(2):
        mvec[0:16, MV_ABF + j] = inp["a_b_f"][j]
    cwt = inp["b_conv_w"][0]
    for j in range(4):
        mvec[:, MV_CW + j * 16:MV_CW + (j + 1) * 16] = col_layout(cwt[j])
    mvec[:, MV_CB:MV_CB + 16] = col_layout(inp["b_conv_b"][0])
    mvec[0:8, MV_BI] = inp["b_b_i"][0]
    mvec[0:8, MV_BF] = inp["b_b_f"][0]
    sh["mvec"] = mvec
    Wb = inp["b_w_in"][0]
    bwin = np.empty((8, 128, KC * 512), np.float32)
    for h in range(8):
        cat = np.concatenate([Wb[:, h * 128:(h + 1) * 128], Wb[:, D + h * 128:D + (h + 1) * 128],
                              Wb[:, 2 * D + h * 128:2 * D + (h + 1) * 128],
                              Wb[:, 3 * D + h * 128:3 * D + (h + 1) * 128]], axis=1)
        bwin[h] = blockify_cols(cat, 512)[0]
    sh["b_win"] = bwin
    sh["b_wg"] = blockify_cols(Wb[:, 4 * D:4 * D + 16], 16)[0]
    sh["b_wout"] = np.ascontiguousarray(inp["b_w_out"][0].reshape(8, 128, D))
    sh["b_ng"] = np.ascontiguousarray(np.broadcast_to(inp["b_norm_g"][0][None, :], (128, D)))
    cst = np.zeros((128, 5, 128), np.float32)
    cst[:, 0, :] = np.eye(128, dtype=np.float32)
    cst[63, 1, :] = 1.0
    kk, qq = np.meshgrid(np.arange(128), np.arange(128), indexing="ij")
    cst[:, 2, :] = np.where(kk > qq, -30000.0, 0.0)
    cst[127, 3, :] = 1.0
    cst[:, 4, :] = (qq >= kk)
    sh["consts"] = cst.reshape(128, 640)
    awin = np.empty((2, 8, 128, KC * 384), np.float32)
    awf = np.empty((2, 128, KC * 16), np.float32)
    for j in range(2):
        W = inp["a_w_in"][j]
        for hp in range(8):
            cat = np.concatenate([W[:, hp * 128:(hp + 1) * 128], W[:, D + hp * 128:D + (hp + 1) * 128],
                                  W[:, 2 * D + hp * 128:2 * D + (hp + 1) * 128]], axis=1)
            awin[j, hp] = blockify_cols(cat, 384)[0]
        awf[j] = blockify_cols(W[:, 3 * D:3 * D + 16], 16)[0]
    sh["a_win"] = awin
    sh["a_wf"] = awf
    sh["a_wout"] = np.ascontiguousarray(inp["a_w_out"].reshape(2, 8, 128, D))
    sh["c_win"] = blockify_cols(inp["c_w_in"][0], 512)
    sh["c_wout"] = np.ascontiguousarray(
        inp["c_w_out"][0].reshape(16, 128, KC, 128).transpose(2, 1, 0, 3)).reshape(KC, 128, 16 * 128)
    sh["c_wsT"] = np.ascontiguousarray(inp["c_w_s"][0].transpose(2, 0, 1)).reshape(128, 1024)
    sh["c_lnb"] = np.ascontiguousarray(inp["c_ln_b"][0].reshape(1, C_W))
    sh["c_bs"] = np.ascontiguousarray(inp["c_b_s"][0].reshape(1, 1024))
    return sh


def stages_from_spec(spec):
    st = []
    for (kind, i) in spec:
        if kind == "ffn":
            st.append(ffn_stage(i))
        else:
            st.append(mixer_stage(i))
    return st


def run(inp, spec, ncores=8):
    inp = {k: np.asarray(v) for k, v in inp.items()}
    nc = build(stages_from_spec(spec))
    sh = prep_shared(inp)
    in_maps = []
    for b in range(ncores):
        m = dict(sh)
        m["xT"] = np.ascontiguousarray(inp["x"][b].T)
        in_maps.append(m)
    res = run_bass_kernel_spmd(nc, in_maps, core_ids=list(range(ncores)))
    return [np.ascontiguousarray(r["outT"].T) for r in res.results]


FULL_SPEC = []
for _i in range(DEPTH):
    FULL_SPEC += [("mix", _i), ("ffn", _i)]


def kernel(**inputs):
    outs = run(inputs, FULL_SPEC, ncores=8)
    return np.stack(outs, axis=0).astype(np.float32)
```

```python
import contextlib
import numpy as np
import concourse.bass as bass
import concourse.mybir as mybir
from concourse.bass_utils import run_bass_kernel_spmd

F32 = mybir.dt.float32
BF16 = mybir.dt.bfloat16
AF = mybir.ActivationFunctionType
ALU = mybir.AluOpType

T = 2048
D = 1024
KC = 8
NTG = 4
DFF = 2816
NF = 22
EPS = 1e-6
DEPTH = 4
SLOT = 4096
NSLOT = 3
ACTT_NF = NF


class Sched:
    def __init__(self, nc):
        self.nc = nc
        self.eng = {"pe": nc.tensor, "act": nc.scalar, "dve": nc.vector, "pool": nc.gpsimd,
                    "sp": nc.sync}
        self.sem = {}
        self.cnt = {}
        self.waited = {}
        self.last_w = {}
        self.reads = {}
        self.pending = {e: False for e in self.eng}
        self.nsem = 0

    def _sem(self, key):
        if key not in self.sem:
            self.sem[key] = self.nc.alloc_semaphore("s_" + str(key))
            self.cnt[key] = 0
            self.nsem += 1
        return self.sem[key]

    def _wait(self, e, deps):
        best = {}
        for (k, v) in deps:
            if v > best.get(k, 0):
                best[k] = v
        for k, v in best.items():
            if k == "pe" and e == "pe":
                continue
            if self.waited.get((e, k), 0) >= v:
                continue
            assert self.cnt[k] >= v, ("dependency on unsignaled instruction", e, k, v, self.cnt[k])
            self.eng[e].wait_ge(self.sem[k], v)
            self.waited[(e, k)] = v

    def _deps(self, r, w):
        deps = []
        for b in r:
            if b in self.last_w:
                deps.append(self.last_w[b])
        for b in w:
            if b in self.last_w:
                deps.append(self.last_w[b])
            deps.extend(self.reads.get(b, ()))
        return deps

    def _record(self, r, w, tag):
        for b in r:
            self.reads.setdefault(b, []).append(tag)
        for b in w:
            self.last_w[b] = tag
            self.reads[b] = []

    def op(self, e, fn, r=(), w=(), sig=True):
        self._sem(e)
        self._wait(e, self._deps(r, w))
        ins = fn(self.eng[e])
        if sig:
            self.cnt[e] += 1
            ins.then_inc(self.sem[e], 1)
            tag = (e, self.cnt[e])
            self.pending[e] = False
        else:
            tag = (e, self.cnt[e] + 1)
            self.pending[e] = True
        self._record(r, w, tag)
        return ins

    def dma(self, q, out, in_, r=(), w=(), chan=None, **kw):
        assert chan is not None
        self._sem(chan)
        self._wait(q, self._deps(r, w))
        ins = self.eng[q].dma_start(out=out, in_=in_, **kw)
        self.cnt[chan] += 16
        ins.then_inc(self.sem[chan], 16)
        self._record(r, w, (chan, self.cnt[chan]))
        return ins

    def barrier(self):
        for e in self.eng:
            assert not self.pending[e]
        keys = [k for k in self.sem if self.cnt[k] > 0]
        for e in self.eng:
            for k in keys:
                if k == e:
                    continue
                if self.waited.get((e, k), 0) >= self.cnt[k]:
                    continue
                self.eng[e].wait_ge(self.sem[k], self.cnt[k])
                self.waited[(e, k)] = self.cnt[k]

    def finish(self, chans):
        for c in chans:
            if c in self.sem and self.cnt[c] > 0:
                self.eng["sp"].wait_ge(self.sem[c], self.cnt[c])


class WPipe:
    def __init__(self, S, slots, depth):
        self.S = S
        self.slots = slots
        self.depth = depth
        self.plan = []
        self.issued = 0
        self.taken = 0
        self.dry = True

    def _issue(self):
        i = self.issued
        ap, n = self.plan[i]
        s = i % len(self.slots)
        self.S.dma("pool", self.slots[s][:, 0:n], ap, w=[("wslot", s)], chan=("wch", s),
                   max_dma_last_dim=2048 * 4)
        self.issued += 1

    def next(self, ap, n):
        if self.dry:
            self.plan.append((ap, n))
            return self.slots[0], ("wslot", 0)
        i = self.taken
        self.taken += 1
        while self.issued < min(len(self.plan), i + self.depth):
            self._issue()
        s = i % len(self.slots)
        return self.slots[s], ("wslot", s)


def blockify_cols(W, cw):
    K, N = W.shape
    kc = K // 128
    nb = N // cw
    a = W.reshape(kc, 128, nb, cw).transpose(2, 1, 0, 3)
    return np.ascontiguousarray(a).reshape(nb, 128, kc * cw)


def col_layout(v):
    return np.ascontiguousarray(v.reshape(-1, 128).T)


class Ctx:
    pass


ARENA_ELEMS = 38 * 1024


class Arena:
    def __init__(self, C):
        self.C = C
        self.off = 0

    def bf(self, n):
        a = self.C.ARENA[:, self.off:self.off + n]
        self.off += n + (n % 2)
        assert self.off <= ARENA_ELEMS, self.off
        return a

    def f32(self, n):
        return self.bf(2 * n).bitcast(F32)


def build(stages, dbg=False):
    nc = bass.Bass("TRN2", target_bir_lowering=False)
    S = Sched(nc)
    C = Ctx()
    C.nc, C.S = nc, S
    es = contextlib.ExitStack()

    def dram(name, shape, kind="ExternalInput", dt=F32):
        return nc.dram_tensor(name, list(shape), dt, kind=kind).ap()

    C.xT = dram("xT", [D, T])
    C.outT = dram("outT", [D, T], kind="ExternalOutput")
    C.vecs = dram("vecs", [128, 80])
    C.ffn_gu = dram("ffn_gu", [DEPTH, NF, 128, KC * 256])
    C.ffn_dn = dram("ffn_dn", [DEPTH, KC, 128, NF * 128])
    declare_mixer_inputs(C, dram)

    with es:
        sb = lambda name, shape, dt: es.enter_context(nc.sbuf_tensor(name, list(shape), dt))
        C.sb = sb
        C.XT = sb("XT", [128, KC, T], F32)
        C.VEC = sb("VEC", [128, 80], F32)
        C.ones_bf = sb("ones_bf", [128, 128], BF16)
        C.epsc = sb("epsc", [128, 1], F32)
        C.slots = [sb("wslot%d" % i, [128, SLOT], BF16) for i in range(NSLOT)]
        C.ARENA = sb("ARENA", [128, ARENA_ELEMS], BF16)
        alloc_mixer_consts(C)
        C.PB = [es.enter_context(nc.psum_tensor("pb%d" % i, [128, 1024], F32)) for i in range(4)]
        C.pbi = 0
        C.W = WPipe(S, C.slots, NSLOT)

        def emit_all():
            prologue(C)
            for st in stages:
                st(C)
            epilogue(C)

        C.W.dry = True
        C.dry = True
        real = (S.op, S.dma, S.barrier)
        S.op = lambda *a, **k: None
        S.dma = lambda *a, **k: None
        S.barrier = lambda *a, **k: None
        emit_all()
        S.op, S.dma, S.barrier = real
        C.W.dry = False
        C.dry = False
        C.pbi = 0
        emit_all()
        S.finish([("och",)])
    return nc


def pbank(C):
    i = C.pbi % 8
    C.pbi += 1
    return C.PB[i // 2][:, (i % 2) * 512:(i % 2 + 1) * 512], ("bk", i)


def pbank2(C):
    if C.pbi % 2:
        C.pbi += 1
    i = C.pbi % 8
    C.pbi += 2
    return C.PB[i // 2][:, :], [("bk", i), ("bk", i + 1)]


def prologue(C):
    S = C.S
    for c in range(KC):
        S.dma("sp", C.XT[:, c, :], C.xT[c * 128:(c + 1) * 128, :],
              w=[("XT", c, n) for n in range(NTG)], chan=("xch", c))
    S.dma("sp", C.VEC[:], C.vecs[:], w=["VEC"], chan="vch")
    S.op("dve", lambda e: e.memset(C.ones_bf[:], 1.0), w=["ones_bf"])
    S.op("dve", lambda e: e.memset(C.epsc[:], EPS), w=["epsc"])
    mixer_prologue(C)


def epilogue(C):
    S = C.S
    S.barrier()
    A = Arena(C)
    tmp = norm_tmp(A)
    rmsnorm(C, 64, lambda c, n: (C.XT[:, c, n * 512:(n + 1) * 512], ("XT", c, n)), range(NTG), tmp)
    for c in range(KC):
        S.dma("sp", C.outT[c * 128:(c + 1) * 128, :], C.XT[:, c, :],
              r=[("XT", c, n) for n in range(NTG)], chan=("och",))


def norm_tmp(A):
    return ([A.bf(512) for _ in range(2)], [A.f32(512) for _ in range(2)])


def rmsnorm(C, gcol, out_fn, groups, tmp):
    S = C.S
    sqs, rss = tmp
    for n in groups:
        ts = slice(n * 512, (n + 1) * 512)
        pb, pk = pbank(C)
        for c in range(KC):
            sq = sqs[c % 2]
            S.op("act", lambda e: e.activation(out=sq, in_=C.XT[:, c, ts], func=AF.Square),
                 r=[("XT", c, n)], w=[("sq", c % 2)])
            S.op("pe", lambda e: e.matmul(pb, C.ones_bf[:], sq, start=(c == 0),
                                          stop=(c == KC - 1)),
                 r=["ones_bf", ("sq", c % 2)], w=[pk], sig=True)
        rs = rss[n % 2]
        rk = ("rs", n % 2)
        S.op("act", lambda e: e.activation(out=rs, in_=pb, func=AF.Sqrt,
                                           scale=1.0 / D, bias=C.epsc[:, 0:1]),
             r=[pk, "epsc"], w=[rk])
        S.op("dve", lambda e: e.reciprocal(out=rs, in_=rs), r=[rk], w=[rk])
        for c in range(KC):
            oap, okey = out_fn(c, n)
            S.op("dve", lambda e: e.scalar_tensor_tensor(
                out=oap, in0=C.XT[:, c, ts], scalar=C.VEC[:, gcol + c:gcol + c + 1], in1=rs,
                op0=ALU.mult, op1=ALU.mult),
                r=[("XT", c, n), "VEC", rk], w=[okey])


def swiglu(C, layer):
    S = C.S
    S.barrier()
    A = Arena(C)
    tmp = norm_tmp(A)
    HTg = A.bf(KC * 1024).rearrange("p (k t) -> p k t", k=KC)
    ACTT = A.bf(NF * 1024).rearrange("p (f t) -> p f t", f=NF)
    sgs = [A.f32(1024) for _ in range(2)]
    for half in range(2):
        t0 = half * 1024
        rmsnorm(C, 32 + 8 * layer,
                lambda c, n: (HTg[:, c, (n % 2) * 512:(n % 2 + 1) * 512], ("HTg", c, n % 2)),
                [2 * half, 2 * half + 1], tmp)
        for f in range(NF):
            wt, wk = C.W.next(C.ffn_gu[layer, f], KC * 256)
            wv = wt[:, 0:KC * 256].rearrange("p (k c) -> p k c", k=KC)
            pg, pgk = pbank2(C)
            pu, puk = pbank2(C)
            for (pp, ppk, off) in ((pg, pgk, 0), (pu, puk, 128)):
                for n2 in range(2):
                    for kc in range(KC):
                        S.op("pe", lambda e: e.matmul(
                            pp[:, n2 * 512:(n2 + 1) * 512], wv[:, kc, off:off + 128],
                            HTg[:, kc, n2 * 512:(n2 + 1) * 512], start=(kc == 0),
                            stop=(kc == KC - 1)),
                            r=[wk, ("HTg", kc, n2)], w=[ppk[n2]], sig=(kc == KC - 1))
            sg = sgs[f % 2]
            sgk = ("sg", f % 2)
            S.op("act", lambda e: e.activation(out=sg, in_=pg[:, :], func=AF.Silu),
                 r=pgk, w=[sgk])
            S.op("dve", lambda e: e.tensor_tensor(out=ACTT[:, f, :], in0=pu[:, :], in1=sg,
                                                  op=ALU.mult),
                 r=puk + [sgk], w=[("ACTT", f)])
        for m in range(KC):
            wt, wk = C.W.next(C.ffn_dn[layer, m], NF * 128)
            wv = wt[:, 0:NF * 128].rearrange("p (f c) -> p f c", f=NF)
            py, pyk = pbank2(C)
            for n2 in range(2):
                for f in range(NF):
                    S.op("pe", lambda e: e.matmul(
                        py[:, n2 * 512:(n2 + 1) * 512], wv[:, f, :],
                        ACTT[:, f, n2 * 512:(n2 + 1) * 512], start=(f == 0), stop=(f == NF - 1)),
                        r=[wk, ("ACTT", f)], w=[pyk[n2]], sig=(f == NF - 1))
            S.op("dve", lambda e: e.tensor_tensor(
                out=C.XT[:, m, t0:t0 + 1024], in0=py[:, :], in1=C.XT[:, m, t0:t0 + 1024],
                op=ALU.add),
                r=pyk + [("XT", m, half * 2), ("XT", m, half * 2 + 1)],
                w=[("XT", m, half * 2), ("XT", m, half * 2 + 1)])


def ffn_stage(layer):
    def st(C):
        swiglu(C, layer)
    return st


C_W = 2048
MV_LNG = 0
MV_ABF = 16
MV_CW = 32
MV_CB = 96
MV_BI = 112
MV_BF = 113
MV_NCOL = 128


def declare_mixer_inputs(C, dram):
    C.mvec = dram("mvec", [128, MV_NCOL])
    C.c_win = dram("c_win", [8, 128, KC * 512])
    C.c_wout = dram("c_wout", [KC, 128, 16 * 128])
    C.c_wsT = dram("c_wsT", [128, 1024])
    C.c_lnb = dram("c_lnb", [1, C_W])
    C.c_bs = dram("c_bs", [1, 1024])
    C.consts = dram("consts", [128, 5 * 128])
    C.a_win = dram("a_win", [2, 8, 128, KC * 384])
    C.a_wf = dram("a_wf", [2, 128, KC * 16])
    C.a_wout = dram("a_wout", [2, 8, 128, D])
    C.b_win = dram("b_win", [8, 128, KC * 512])
    C.b_wg = dram("b_wg", [128, KC * 16])
    C.b_wout = dram("b_wout", [8, 128, D])
    C.b_ng = dram("b_ng", [128, D])


def alloc_mixer_consts(C):
    C.MVEC = C.sb("MVEC", [128, MV_NCOL], F32)
    C.CONST = C.sb("CONST", [128, 5 * 128], F32)
    C.ident_bf = C.sb("ident_bf", [128, 128], BF16)
    C.negmask_bf = C.sb("negmask_bf", [128, 128], BF16)


def mixer_prologue(C):
    C.S.dma("sp", C.MVEC[:], C.mvec[:], w=["MVEC"], chan="vch")
    C.S.dma("sp", C.CONST[:], C.consts[:], w=["CONST"], chan="vch")
    C.S.dma("pool", C.ident_bf[:], C.consts[:, 0:128], w=["ident_bf"], chan="cch")
    C.S.dma("pool", C.negmask_bf[:], C.consts[:, 256:384], w=["negmask_bf"], chan="cch")
    C.ident = C.CONST[:, 0:128]
    C.sel63 = C.CONST[:, 128:256]
    C.negmask = C.CONST[:, 256:384]
    C.sel127 = C.CONST[:, 384:512]
    C.tri01 = C.CONST[:, 512:640]


def gmlp(C, layer):
    S = C.S
    S.barrier()
    A = Arena(C)
    tmp = norm_tmp(A)
    HTg = A.bf(KC * 512).rearrange("p (k t) -> p k t", k=KC)
    Vtok = A.bf(4 * C_W).rearrange("p (n f) -> p n f", n=4)
    UT = A.bf(16 * 512).rearrange("p (f t) -> p f t", f=16)
    junk = A.bf(512)
    tmps = [A.f32(512) for _ in range(2)]
    E = A.f32(16 * 128).rearrange("p (f t) -> p f t", f=16)
    wsT = A.bf(1024)
    LB2 = A.f32(C_W)
    R2 = A.f32(1024)
    S1 = A.f32(16)
    S2 = A.f32(16)
    st4 = A.f32(16)

    S.dma("pool", wsT, C.c_wsT[:], w=["wsT"], chan="cch")
    S.op("dve", lambda e: e.memset(wsT[64:128, :].rearrange("p (g t) -> p g t", g=8)[:, :, 0:64],
                                   0.0), r=[], w=["wsT"])
    S.op("dve", lambda e: e.memset(LB2[0:2, :], 1.0), w=["LB2"])
    S.dma("sp", LB2[0:1, :], C.c_lnb[:], w=["LB2"], chan="cch2")
    S.dma("sp", R2[1:2, :], C.c_bs[:], w=["R2b"], chan="cch3")
    for i in range(2):
        pb, pk = pbank(C)
        S.op("pe", lambda e: e.matmul(pb[0:1, :], C.ones_bf[:, 0:1],
                                      wsT[:, i * 512:(i + 1) * 512], start=True, stop=True),
             r=["wsT", "ones_bf"], w=[pk])
        S.op("act", lambda e: e.activation(out=R2[0:1, i * 512:(i + 1) * 512], in_=pb[0:1, :],
                                           func=AF.Copy), r=[pk], w=["R2a%d" % i])
    for fc in range(16):
        g = fc // 2
        if fc % 4 == 0:
            pb, pk = pbank(C)
        S.op("pe", lambda e: e.matmul(pb[:, (fc % 4) * 128:(fc % 4 + 1) * 128],
                                      LB2[0:2, fc * 128:(fc + 1) * 128],
                                      R2[0:2, g * 128:(g + 1) * 128], start=True, stop=True),
             r=["LB2", "R2b", "R2a0", "R2a1"], w=[pk])
        if fc % 4 == 3:
            f0 = fc - 3
            S.op("act", lambda e: e.activation(
                out=E[:, f0:f0 + 4, :], in_=pb.rearrange("p (f t) -> p f t", f=4),
                func=AF.Copy), r=[pk], w=["E"])

    for tg in range(NTG):
        rmsnorm(C, 8 * layer, lambda c, n: (HTg[:, c, :], ("HTg", c)), [tg], tmp)
        for j in range(4):
            wt, wk = C.W.next(C.c_win[j], KC * 512)
            wv = wt[:, 0:KC * 512].rearrange("p (k c) -> p k c", k=KC)
            for q in range(4):
                fc = j * 4 + q
                po, pk = pbank(C)
                for kc in range(KC):
                    S.op("pe", lambda e: e.matmul(po, wv[:, kc, q * 128:(q + 1) * 128],
                                                  HTg[:, kc, :], start=(kc == 0),
                                                  stop=(kc == KC - 1)),
                         r=[wk, ("HTg", kc)], w=[pk], sig=(kc == KC - 1))
                S.op("act", lambda e: e.activation(out=UT[:, fc, :], in_=po, func=AF.Gelu),
                     r=[pk], w=[("UT", fc)])
        for j in range(4):
            wt, wk = C.W.next(C.c_win[4 + j], KC * 512)
            wv = wt[:, 0:KC * 512].rearrange("p (k c) -> p k c", k=KC)
            for n in range(4):
                po, pk = pbank(C)
                for kc in range(KC):
                    S.op("pe", lambda e: e.matmul(po, HTg[:, kc, n * 128:(n + 1) * 128],
                                                  wv[:, kc, :], start=(kc == 0),
                                                  stop=(kc == KC - 1)),
                         r=[wk, ("HTg", kc)], w=[pk], sig=(kc == KC - 1))
                vb = Vtok[:, n, j * 512:(j + 1) * 512]
                col = n * 4 + j
                S.op("act", lambda e: e.activation(out=vb, in_=po, func=AF.Gelu,
                                                   accum_out=S1[:, col:col + 1]),
                     r=[pk], w=[("Vtok", n, j), ("S1", col)])
                S.op("act", lambda e: e.activation(out=junk, in_=vb, func=AF.Square,
                                                   accum_out=S2[:, col:col + 1]),
                     r=[("Vtok", n, j)], w=["junk", ("S2", col)])
        allS1 = [("S1", c) for c in range(16)]
        allS2 = [("S2", c) for c in range(16)]
        S.op("dve", lambda e: e.tensor_reduce(out=st4[:, 0:4],
                                              in_=S1.rearrange("p (n j) -> p n j", n=4),
                                              op=ALU.add, axis=mybir.AxisListType.X),
             r=allS1, w=["st_mu"])
        S.op("dve", lambda e: e.tensor_reduce(out=st4[:, 4:8],
                                              in_=S2.rearrange("p (n j) -> p n j", n=4),
                                              op=ALU.add, axis=mybir.AxisListType.X),
             r=allS2, w=["st_ex2"])
        S.op("dve", lambda e: e.tensor_scalar(out=st4[:, 0:8], in0=st4[:, 0:8], scalar1=1.0 / C_W,
                                              scalar2=None, op0=ALU.mult),
             r=["st_mu", "st_ex2"], w=["st_mu", "st_ex2"])
        S.op("dve", lambda e: e.tensor_tensor(out=st4[:, 8:12], in0=st4[:, 0:4], in1=st4[:, 0:4],
                                              op=ALU.mult), r=["st_mu"], w=["st_var"])
        S.op("dve", lambda e: e.tensor_tensor(out=st4[:, 8:12], in0=st4[:, 4:8], in1=st4[:, 8:12],
                                              op=ALU.subtract), r=["st_ex2", "st_var"], w=["st_var"])
        S.op("act", lambda e: e.activation(out=st4[:, 8:12], in_=st4[:, 8:12], func=AF.Sqrt,
                                           bias=C.epsc[:, 0:1]), r=["st_var", "epsc"], w=["st_var"])
        S.op("dve", lambda e: e.reciprocal(out=st4[:, 12:16], in_=st4[:, 8:12]),
             r=["st_var"], w=["st_rstd"])
        for n in range(4):
            S.op("dve", lambda e: e.tensor_scalar(
                out=Vtok[:, n, :], in0=Vtok[:, n, :], scalar1=st4[:, n:n + 1],
                scalar2=st4[:, 12 + n:13 + n], op0=ALU.subtract, op1=ALU.mult),
                r=["st_mu", "st_rstd"] + [("Vtok", n, j) for j in range(4)],
                w=[("Vtok", n, j) for j in range(4)])
        for fc in range(16):
            g = fc // 2
            pb, pk = pbank(C)
            for n in range(4):
                S.op("pe", lambda e: e.matmul(pb[:, n * 128:(n + 1) * 128],
                                              Vtok[:, n, fc * 128:(fc + 1) * 128],
                                              wsT[:, g * 128:(g + 1) * 128], start=True, stop=True),
                     r=["wsT"] + [("Vtok", n, j) for j in range(4)], w=[pk], sig=(n == 3))
            tm = tmps[fc % 2]
            tk = ("tmp", fc % 2)
            S.op("dve", lambda e: e.scalar_tensor_tensor(
                out=tm.rearrange("p (n t) -> p n t", n=4),
                in0=pb.rearrange("p (n t) -> p n t", n=4),
                scalar=C.MVEC[:, MV_LNG + fc:MV_LNG + fc + 1],
                in1=E[:, fc:fc + 1, :].to_broadcast([128, 4, 128]),
                op0=ALU.mult, op1=ALU.add), r=[pk, "MVEC", "E"], w=[tk])
            S.op("dve", lambda e: e.tensor_tensor(out=UT[:, fc, :], in0=tm, in1=UT[:, fc, :],
                                                  op=ALU.mult), r=[tk, ("UT", fc)], w=[("UT", fc)])
        for m in range(KC):
            wt, wk = C.W.next(C.c_wout[m], 16 * 128)
            wv = wt[:, 0:16 * 128].rearrange("p (f c) -> p f c", f=16)
            po, pk = pbank(C)
            for fc in range(16):
                S.op("pe", lambda e: e.matmul(po, wv[:, fc, :], UT[:, fc, :], start=(fc == 0),
                                              stop=(fc == 15)),
                     r=[wk, ("UT", fc)], w=[pk], sig=(fc == 15))
            S.op("dve", lambda e: e.tensor_tensor(
                out=C.XT[:, m, tg * 512:(tg + 1) * 512], in0=po,
                in1=C.XT[:, m, tg * 512:(tg + 1) * 512], op=ALU.add),
                r=[pk, ("XT", m, tg)], w=[("XT", m, tg)])


def bank(C, i):
    return C.PB[i // 2][:, (i % 2) * 512:(i % 2 + 1) * 512], ("bk", i)


def fox(C, layer):
    S = C.S
    j_ = layer // 3
    S.barrier()
    A = Arena(C)
    HT = A.bf(KC * T).rearrange("p (k t) -> p k t", k=KC)
    a_tok = A.f32(256)
    amid = A.f32(256)
    negb = A.f32(2)
    QTz = [A.bf(T) for _ in range(2)]
    KTa = [A.bf(T) for _ in range(2)]
    Vtm = A.bf(16 * 128).rearrange("p (n e) -> p n e", n=16)
    augrow = [QTz[0][64:65, :], QTz[1][0:1, :]]
    mark = A.off
    tmp = norm_tmp(A)
    A.off = mark
    sp = A.f32(T)
    acs = A.f32(T)
    onesrow = A.bf(T)
    A.off = mark
    PTs = [A.bf(512) for _ in range(3)]
    OT = A.bf(T)
    rec = [A.f32(512) for _ in range(2)]

    rmsnorm(C, 8 * layer, lambda c, n: (HT[:, c, n * 512:(n + 1) * 512], ("HT", c, n)),
            range(NTG), tmp)
    S.barrier()

    wt, wk = C.W.next(C.a_wf[j_], KC * 16)
    wv = wt[:, 0:KC * 16].rearrange("p (k c) -> p k c", k=KC)
    S.op("dve", lambda e: e.tensor_scalar(out=negb[0:16, 0:1],
                                          in0=C.MVEC[0:16, MV_ABF + j_:MV_ABF + j_ + 1],
                                          scalar1=-1.0, scalar2=None, op0=ALU.mult),
         r=["MVEC"], w=["negb"])
    S.op("dve", lambda e: e.memset(onesrow[0:16, :], 1.0), w=["onesrow"])
    S.op("dve", lambda e: e.memset(negb[0:16, 1:2], 1.0), w=["one1"])
    for n in range(NTG):
        pb, pk = pbank(C)
        for kc in range(KC):
            S.op("pe", lambda e: e.matmul(pb[0:16, :], wv[:, kc, :], HT[:, kc, n * 512:(n + 1) * 512],
                                          start=(kc == 0), stop=(kc == KC - 1)),
                 r=[wk, ("HT", kc, n)], w=[pk], sig=(kc == KC - 1))
        S.op("act", lambda e: e.activation(out=sp[0:16, n * 512:(n + 1) * 512], in_=pb[0:16, :],
                                           func=AF.Exp, scale=-1.0, bias=negb[0:16, 0:1]),
             r=[pk, "negb"], w=[("sp", n)])
    S.op("act", lambda e: e.activation(out=sp[0:16, :], in_=sp[0:16, :], func=AF.Ln,
                                       bias=negb[0:16, 1:2]),
         r=[("sp", n) for n in range(NTG)] + ["one1"], w=["spl"])
    S.op("dve", lambda e: e.tensor_tensor_scan(out=acs[0:16, :], data0=onesrow[0:16, :],
                                               data1=sp[0:16, :], initial=0.0,
                                               op0=ALU.mult, op1=ALU.add),
         r=["spl", "onesrow"], w=["acs"])
    pb, pk = pbank(C)
    for n in range(16):
        S.op("pe", lambda e: e.transpose(pb[:, n * 16:(n + 1) * 16], acs[0:16, n * 128:(n + 1) * 128],
                                         C.ident[0:16, 0:16]),
             r=["acs", "CONST"], w=[pk], sig=(n == 15))
    S.op("dve", lambda e: e.tensor_copy(out=a_tok, in_=pb[:, 0:256]), r=[pk], w=["a_tok"])
    pb, pk = pbank(C)
    S.op("pe", lambda e: e.matmul(pb[:, 0:256], C.sel63, a_tok, start=True, stop=True),
         r=["a_tok", "CONST"], w=[pk])
    S.op("dve", lambda e: e.tensor_copy(out=amid, in_=pb[:, 0:256]), r=[pk], w=["amid"])
    S.barrier()

    m3 = amid.rearrange("p (t h) -> p h t", h=16)
    scale = 64 ** -0.5
    BK_ST, BK_OT, BK_DEN, BK_MISC = (0, 1), (2, 3), (4, 5), (6, 7)
    misc_i = [0]

    def misc_bank():
        b = BK_MISC[misc_i[0] % 2]
        misc_i[0] += 1
        return bank(C, b)

    S.op("dve", lambda e: e.memset(QTz[0][64:128, :], 0.0), w=[("QTpad", 0)])
    S.op("dve", lambda e: e.memset(QTz[1][0:64, :], 0.0), w=[("QTpad", 1)])
    S.op("dve", lambda e: e.memset(KTa[0][64:128, :], 0.0), w=[("KTpad", 0)])
    S.op("dve", lambda e: e.memset(KTa[1][0:64, :], 0.0), w=[("KTpad", 1)])
    S.op("dve", lambda e: e.memset(KTa[0][64:65, :], 1.0), r=[("KTpad", 0)], w=[("KTpad", 0)])
    S.op("dve", lambda e: e.memset(KTa[1][0:1, :], 1.0), r=[("KTpad", 1)], w=[("KTpad", 1)])

    for hp in range(8):
        wt, wk = C.W.next(C.a_win[j_, hp], KC * 384)
        wv = wt[:, 0:KC * 384].rearrange("p (k c) -> p k c", k=KC)
        for (dst, off, key) in ((QTz, 0, "QT"), (KTa, 128, "KT")):
            for n in range(NTG):
                pb, pk = misc_bank()
                for kc in range(KC):
                    S.op("pe", lambda e: e.matmul(pb, wv[:, kc, off:off + 128],
                                                  HT[:, kc, n * 512:(n + 1) * 512],
                                                  start=(kc == 0), stop=(kc == KC - 1)),
                         r=[wk, ("HT", kc, n)], w=[pk], sig=(kc == KC - 1))
                for hh in range(2):
                    p0 = hh * 64
                    S.op("dve", lambda e: e.tensor_copy(
                        out=dst[hh][p0:p0 + 64, n * 512:(n + 1) * 512], in_=pb[p0:p0 + 64, :]),
                        r=[pk], w=[(key, hh, n)])
        for n4 in range(4):
            pb, pk = misc_bank()
            for i in range(4):
                n = n4 * 4 + i
                for kc in range(KC):
                    S.op("pe", lambda e: e.matmul(pb[:, i * 128:(i + 1) * 128],
                                                  HT[:, kc, n * 128:(n + 1) * 128],
                                                  wv[:, kc, 256:384],
                                                  start=(kc == 0), stop=(kc == KC - 1)),
                         r=[wk, ("HT", kc, n // 4)], w=[pk], sig=(kc == KC - 1 and i == 3))
            S.op("act", lambda e: e.activation(
                out=Vtm[:, n4 * 4:n4 * 4 + 4, :],
                in_=pb.rearrange("p (n e) -> p n e", n=4), func=AF.Copy),
                r=[pk], w=[("V", n4)])
        for hh in range(2):
            h = hp * 2 + hh
            pr = 64 if hh == 0 else 0
            S.op("dve", lambda e: e.tensor_scalar(
                out=augrow[hh].rearrange("p (j t) -> p j t", j=16),
                in0=m3[pr:pr + 1, h, :].rearrange("p (j o) -> p j o", o=1)
                .to_broadcast([1, 16, 128]),
                scalar1=-1.0 / scale, scalar2=None, op0=ALU.mult),
                r=["amid", ("QTpad", hh)], w=[("aug", hh)])

        steps = [(hh, Q, kb) for hh in range(2) for Q in range(4) for kb in range(4 * Q + 4)]

        def emit_scores(idx):
            hh, Q, kb = steps[idx]
            h = hp * 2 + hh
            i = max(0, kb - 4 * Q)
            q0 = Q * 512 + i * 128
            N = 512 - i * 128
            diag = kb >= 4 * Q
            st, stk = bank(C, BK_ST[idx % 2])
            S.op("pe", lambda e: e.matmul(st[:, 0:N], KTa[hh][:, kb * 128:(kb + 1) * 128],
                                          QTz[hh][:, q0:q0 + N], start=True, stop=not diag),
                 r=[("KT", hh, kb // 4), ("KTpad", hh), ("QT", hh, Q), ("QTpad", hh), ("aug", hh)],
                 w=[stk], sig=not diag)
            if diag:
                S.op("pe", lambda e: e.matmul(st[:, 0:128], C.ident_bf[:], C.negmask_bf[:],
                                              start=False, stop=True),
                     r=["ident_bf", "negmask_bf"], w=[stk])
            P = PTs[idx % 3]
            S.op("act", lambda e: e.activation(out=P[:, 0:N], in_=st[:, 0:N], func=AF.Exp,
                                               scale=scale,
                                               bias=a_tok[:, kb * 16 + h:kb * 16 + h + 1]),
                 r=[stk, "a_tok"], w=[("PT", idx % 3)])

        def emit_pv(idx):
            hh, Q, kb = steps[idx]
            p0 = hh * 64
            i = max(0, kb - 4 * Q)
            N = 512 - i * 128
            g = hh * 4 + Q
            ot, otk = bank(C, BK_OT[g % 2])
            dn, dnk = bank(C, BK_DEN[g % 2])
            P = PTs[idx % 3]
            last = (kb == 4 * Q + 3)
            S.op("pe", lambda e: e.matmul(ot[p0:p0 + 64, i * 128:512], Vtm[:, kb, p0:p0 + 64],
                                          P[:, 0:N], start=(kb == 0), stop=last),
                 r=[("V", kb // 4), ("PT", idx % 3)], w=[otk], sig=False)
            S.op("pe", lambda e: e.matmul(dn[p0:p0 + 64, i * 128:512], C.ones_bf[:, 0:64],
                                          P[:, 0:N], start=(kb == 0), stop=last),
                 r=["ones_bf", ("PT", idx % 3)], w=[dnk])
            if last:
                rc = rec[g % 2]
                S.op("dve", lambda e: e.reciprocal(out=rc[p0:p0 + 64, :], in_=dn[p0:p0 + 64, :]),
                     r=[dnk], w=[("rec", g % 2)])
                S.op("dve", lambda e: e.tensor_tensor(
                    out=OT[p0:p0 + 64, Q * 512:(Q + 1) * 512], in0=ot[p0:p0 + 64, :],
                    in1=rc[p0:p0 + 64, :], op=ALU.mult),
                    r=[otk, ("rec", g % 2)], w=[("OT", Q)])

        for idx in range(len(steps)):
            emit_scores(idx)
            if idx >= 1:
                emit_pv(idx - 1)
        emit_pv(len(steps) - 1)

        wt, wk = C.W.next(C.a_wout[j_, hp], D)
        for m in range(KC):
            for n in range(NTG):
                pb, pk = pbank(C)
                S.op("pe", lambda e: e.matmul(pb, wt[:, m * 128:(m + 1) * 128],
                                              OT[:, n * 512:(n + 1) * 512], start=True, stop=True),
                     r=[wk, ("OT", n)], w=[pk])
                S.op("dve", lambda e: e.tensor_tensor(
                    out=C.XT[:, m, n * 512:(n + 1) * 512], in0=pb,
                    in1=C.XT[:, m, n * 512:(n + 1) * 512], op=ALU.add),
                    r=[pk, ("XT", m, n)], w=[("XT", m, n)])


def mlstm(C, layer):
    S = C.S
    S.barrier()
    A = Arena(C)
    HT = A.bf(KC * T).rearrange("p (k t) -> p k t", k=KC)
    GTok = A.f32(384).rearrange("p (a t h) -> p a t h", a=3, t=16)
    Mend = A.f32(128)
    wkk = A.f32(128)
    uu = A.f32(128)
    dec = A.f32(128)
    enm = A.f32(128)
    NGh = A.f32(128)
    gsm = A.f32(8)
    mark = A.off
    tmp = norm_tmp(A)
    rmsnorm(C, 8 * layer, lambda c, n: (HT[:, c, n * 512:(n + 1) * 512], ("HT", c, n)),
            range(NTG), tmp)
    S.barrier()
    A.off = mark
    T1 = A.f32(T)
    T2 = A.f32(T)
    T3 = A.f32(T)
    onesrow = A.bf(T)

    wt, wk = C.W.next(C.b_wg[:], KC * 16)
    wv = wt[:, 0:KC * 16].rearrange("p (k c) -> p k c", k=KC)
    S.op("dve", lambda e: e.tensor_scalar(out=gsm[0:8, 0:1], in0=C.MVEC[0:8, MV_BF:MV_BF + 1],
                                          scalar1=-1.0, scalar2=None, op0=ALU.mult),
         r=["MVEC"], w=["gsm0"])
    S.op("dve", lambda e: e.memset(gsm[0:8, 1:2], 1.0), w=["gsm1"])
    S.op("dve", lambda e: e.memset(onesrow[0:8, :], 1.0), w=["onesrow"])
    igp = []
    for n in range(NTG):
        pi, pik = pbank(C)
        pf, pfk = pbank(C)
        for (pp, ppk, off) in ((pi, pik, 0), (pf, pfk, 8)):
            for kc in range(KC):
                S.op("pe", lambda e: e.matmul(pp[0:8, :], wv[:, kc, off:off + 8],
                                              HT[:, kc, n * 512:(n + 1) * 512],
                                              start=(kc == 0), stop=(kc == KC - 1)),
                     r=[wk, ("HT", kc, n)], w=[ppk], sig=(kc == KC - 1))
        S.op("act", lambda e: e.activation(out=T1[0:8, n * 512:(n + 1) * 512], in_=pf[0:8, :],
                                           func=AF.Exp, scale=-1.0, bias=gsm[0:8, 0:1]),
             r=[pfk, "gsm0"], w=[("T1", n)])
        S.op("dve", lambda e: e.tensor_scalar(out=T3[0:8, n * 512:(n + 1) * 512], in0=pi[0:8, :],
                                              scalar1=C.MVEC[0:8, MV_BI:MV_BI + 1], scalar2=None,
                                              op0=ALU.add), r=[pik, "MVEC"], w=[("T3", n)])
    allT = lambda nm: [(nm, n) for n in range(NTG)]
    S.op("act", lambda e: e.activation(out=T1[0:8, :], in_=T1[0:8, :], func=AF.Ln,
                                       bias=gsm[0:8, 1:2]), r=allT("T1") + ["gsm1"], w=allT("T1"))
    S.op("dve", lambda e: e.tensor_tensor_scan(out=T2[0:8, :], data0=onesrow[0:8, :],
                                               data1=T1[0:8, :], initial=0.0,
                                               op0=ALU.mult, op1=ALU.add),
         r=allT("T1") + ["onesrow"], w=["T2"])
    S.op("dve", lambda e: e.tensor_tensor(out=T1[0:8, :], in0=T3[0:8, :], in1=T2[0:8, :],
                                          op=ALU.add), r=allT("T3") + ["T2"], w=allT("T1"))
    S.op("dve", lambda e: e.tensor_tensor_scan(out=T3[0:8, :], data0=T1[0:8, :], data1=T1[0:8, :],
                                               initial=0.0, op0=ALU.max, op1=ALU.max),
         r=allT("T1"), w=allT("T3"))
    S.op("dve", lambda e: e.tensor_tensor(out=T2[0:8, :], in0=T2[0:8, :], in1=T3[0:8, :],
                                          op=ALU.subtract), r=["T2"] + allT("T3"), w=["T2"])
    pb, pk = pbank(C)
    srcs = (T1, T3, T2)
    for a_ in range(3):
        for n in range(16):
            col = (a_ * 16 + n) * 8
            S.op("pe", lambda e: e.transpose(pb[:, col:col + 8],
                                             srcs[a_][0:8, n * 128:(n + 1) * 128],
                                             C.ident[0:8, 0:8]),
                 r=allT("T1") + allT("T3") + ["T2", "CONST"], w=[pk], sig=(a_ == 2 and n == 15))
    S.op("dve", lambda e: e.tensor_copy(out=GTok.rearrange("p a t h -> p (a t h)"),
                                        in_=pb[:, 0:384]), r=[pk], w=["GTok"])
    Rt = GTok[:, 0].rearrange("p t h -> p (t h)")
    Mt = GTok[:, 1].rearrange("p t h -> p (t h)")
    NMt = GTok[:, 2].rearrange("p t h -> p (t h)")
    pb, pk = pbank(C)
    S.op("pe", lambda e: e.matmul(pb[:, 0:128], C.sel127, Mt, start=True, stop=True),
         r=["GTok", "CONST"], w=[pk])
    S.op("dve", lambda e: e.tensor_copy(out=Mend, in_=pb[:, 0:128]), r=[pk], w=["Mend"])
    kscale = 128 ** -0.5
    S.op("dve", lambda e: e.tensor_tensor(out=wkk, in0=Rt, in1=Mend, op=ALU.subtract),
         r=["GTok", "Mend"], w=["wkk"])
    S.op("act", lambda e: e.activation(out=wkk, in_=wkk, func=AF.Exp), r=["wkk"], w=["wkk"])
    S.op("dve", lambda e: e.tensor_scalar(out=wkk, in0=wkk, scalar1=kscale, scalar2=None,
                                          op0=ALU.mult), r=["wkk"], w=["wkk"])
    S.op("dve", lambda e: e.tensor_tensor(out=uu, in0=Mend, in1=Mt, op=ALU.subtract),
         r=["GTok", "Mend"], w=["uu"])
    S.op("act", lambda e: e.activation(out=uu, in_=uu, func=AF.Exp), r=["uu"], w=["uu"])
    S.op("act", lambda e: e.activation(out=enm, in_=NMt, func=AF.Exp), r=["GTok"], w=["enm"])
    S.op("dve", lambda e: e.tensor_scalar(out=dec[:, 0:8], in0=Mend[:, 0:8], scalar1=-1.0,
                                          scalar2=None, op0=ALU.mult), r=["Mend"], w=["dec0"])
    S.op("dve", lambda e: e.tensor_tensor(out=dec[:, 8:128], in0=Mend[:, 0:120],
                                          in1=Mend[:, 8:128], op=ALU.subtract),
         r=["Mend"], w=["dec1"])
    S.op("act", lambda e: e.activation(out=dec, in_=dec, func=AF.Exp), r=["dec0", "dec1"],
         w=["dec"])
    S.barrier()
    A.off = mark
    QT = A.bf(T)
    KT = A.bf(T)
    Ktok = A.bf(16 * 128).rearrange("p (n f) -> p n f", n=16)
    Vaug = A.bf(16 * 130).rearrange("p (n e) -> p n e", n=16)
    OG = A.bf(16 * 128).rearrange("p (n e) -> p n e", n=16)
    Htok = KT.rearrange("p (n e) -> p n e", n=16)
    HR = A.f32(16 * 130).rearrange("p (n e) -> p n e", n=16)
    ep = A.f32(6 * 16).rearrange("p (a c) -> p a c", a=6)
    uu3 = uu.rearrange("p (c h) -> p c h", h=8)
    enm3 = enm.rearrange("p (c h) -> p c h", h=8)
    XPs = [A.f32(516) for _ in range(2)]
    cts = [A.f32(512) for _ in range(2)]
    Pm = [A.bf(128) for _ in range(2)]
    Vt = [A.bf(130) for _ in range(2)]
    Sst = [A.f32(130) for _ in range(2)]
    Sbf = [A.bf(130) for _ in range(2)]
    junk = A.bf(128)
    S.op("dve", lambda e: e.memset(Vaug[:, :, 128:129], 1.0), w=["Vones"])

    for h in range(8):
        wt, wk = C.W.next(C.b_win[h], KC * 512)
        wv = wt[:, 0:KC * 512].rearrange("p (k c) -> p k c", k=KC)
        S.dma("sp", NGh, C.b_ng[:, h * 128:(h + 1) * 128], w=["NG"], chan="cch2")
        for (dst, off, ch, key) in ((QT, 0, h, "QT"), (KT, 128, 8 + h, "KT")):
            S.op("dve", lambda e: e.memset(XPs[0][:, 0:3], 0.0), r=[], w=[("XPh", 0)])
            cw = lambda j: C.MVEC[:, MV_CW + j * 16 + ch:MV_CW + j * 16 + ch + 1]
            for n in range(NTG):
                XPg, ctmp = XPs[n % 2], cts[n % 2]
                xk, xh, ck = ("XPg", n % 2), ("XPh", n % 2), ("ctmp", n % 2)
                pb, pk = pbank(C)
                for kc in range(KC):
                    S.op("pe", lambda e: e.matmul(pb, wv[:, kc, off:off + 128],
                                                  HT[:, kc, n * 512:(n + 1) * 512],
                                                  start=(kc == 0), stop=(kc == KC - 1)),
                         r=[wk, ("HT", kc, n)], w=[pk], sig=(kc == KC - 1))
                S.op("act", lambda e: e.activation(out=XPg[:, 3:515], in_=pb, func=AF.Copy),
                     r=[pk], w=[xk])
                S.op("act", lambda e: e.activation(
                    out=ctmp, in_=pb, func=AF.Identity, scale=cw(3),
                    bias=C.MVEC[:, MV_CB + ch:MV_CB + ch + 1]),
                    r=[pk, "MVEC"], w=[ck])
                if n < NTG - 1:
                    S.op("act", lambda e: e.activation(out=XPs[(n + 1) % 2][:, 0:3],
                                                       in_=XPg[:, 512:515], func=AF.Copy),
                         r=[xk], w=[("XPh", (n + 1) % 2)])
                for j in (2, 1, 0):
                    S.op("dve", lambda e: e.scalar_tensor_tensor(
                        out=ctmp, in0=XPg[:, j:j + 512], scalar=cw(j), in1=ctmp,
                        op0=ALU.mult, op1=ALU.add), r=[xk, xh, "MVEC", ck], w=[ck])
                S.op("act", lambda e: e.activation(out=dst[:, n * 512:(n + 1) * 512], in_=ctmp,
                                                   func=AF.Silu), r=[ck], w=[(key, n)])
        for n4 in range(4):
            pb, pk = pbank(C)
            pbb = pb.bitcast(BF16)
            for i in range(4):
                n = n4 * 4 + i
                S.op("pe", lambda e: e.transpose(pbb[:, i * 128:(i + 1) * 128],
                                                 KT[:, n * 128:(n + 1) * 128], C.ident_bf[:]),
                     r=[("KT", n4), "ident_bf"], w=[pk], sig=(i == 3))
            S.op("act", lambda e: e.activation(
                out=Ktok[:, n4 * 4:n4 * 4 + 4, :],
                in_=pbb[:, 0:512].rearrange("p (n f) -> p n f", n=4), func=AF.Copy),
                r=[pk], w=[("Ktok", n4)])
        for n4 in range(4):
            pv, pvk = pbank(C)
            po_, pok_ = pbank(C)
            for (pp, ppk, off) in ((pv, pvk, 256), (po_, pok_, 384)):
                for i in range(4):
                    n = n4 * 4 + i
                    for kc in range(KC):
                        S.op("pe", lambda e: e.matmul(pp[:, i * 128:(i + 1) * 128],
                                                      HT[:, kc, n * 128:(n + 1) * 128],
                                                      wv[:, kc, off:off + 128],
                                                      start=(kc == 0), stop=(kc == KC - 1)),
                             r=[wk, ("HT", kc, n4)], w=[ppk], sig=(kc == KC - 1 and i == 3))
            S.op("act", lambda e: e.activation(
                out=Vaug[:, n4 * 4:n4 * 4 + 4, 0:128],
                in_=pv.rearrange("p (n e) -> p n e", n=4), func=AF.Copy),
                r=[pvk, "Vones"], w=[("V", n4)])
            S.op("act", lambda e: e.activation(
                out=OG[:, n4 * 4:n4 * 4 + 4, :], in_=po_.rearrange("p (n e) -> p n e", n=4),
                func=AF.Sigmoid), r=[pok_], w=[("OG", n4)])
            S.op("dve", lambda e: e.tensor_tensor(
                out=OG[:, n4 * 4:n4 * 4 + 4, :], in0=OG[:, n4 * 4:n4 * 4 + 4, :],
                in1=NGh.rearrange("p (o e) -> p o e", o=1).to_broadcast([128, 4, 128]),
                op=ALU.mult),
                r=[("OG", n4), "NG"], w=[("OG", n4)])
        for c in range(16):
            col = c * 8 + h
            cs = slice(c * 128, (c + 1) * 128)
            ps_, psk = pbank(C)
            S.op("pe", lambda e: e.matmul(ps_[:, 0:128], KT[:, cs], QT[:, cs], start=True,
                                          stop=True), r=[("KT", c // 4), ("QT", c // 4)], w=[psk])
            P = Pm[c % 2]
            S.op("dve", lambda e: e.scalar_tensor_tensor(
                out=P, in0=ps_[:, 0:128], scalar=wkk[:, col:col + 1], in1=C.tri01,
                op0=ALU.mult, op1=ALU.mult), r=[psk, "wkk", "CONST"], w=[("P", c % 2)])
            V_ = Vt[c % 2]
            S.op("dve", lambda e: e.tensor_scalar(out=V_[:, 0:129], in0=Vaug[:, c, 0:129],
                                                  scalar1=wkk[:, col:col + 1], scalar2=None,
                                                  op0=ALU.mult),
                 r=[("V", c // 4), "Vones", "wkk"], w=[("Vt", c % 2)])
            pu_, puk_ = pbank(C)
            S.op("pe", lambda e: e.matmul(pu_[:, 0:129], Ktok[:, c, :], V_[:, 0:129], start=True,
                                          stop=True), r=[("Ktok", c // 4), ("Vt", c % 2)],
                 w=[puk_])
            Sc, Sn = Sst[c % 2], Sst[(c + 1) % 2]
            if c == 0:
                S.op("dve", lambda e: e.tensor_copy(out=Sn[:, 0:129], in_=pu_[:, 0:129]),
                     r=[puk_], w=[("Sst", (c + 1) % 2)])
            else:
                S.op("dve", lambda e: e.scalar_tensor_tensor(
                    out=Sn[:, 0:129], in0=Sc[:, 0:129], scalar=dec[:, col:col + 1],
                    in1=pu_[:, 0:129], op0=ALU.mult, op1=ALU.add),
                    r=[puk_, ("Sst", c % 2), "dec"], w=[("Sst", (c + 1) % 2)])
            po, pok = pbank(C)
            if c > 0:
                Sb = Sbf[c % 2]
                S.op("act", lambda e: e.activation(out=Sb[:, 0:129], in_=Sc[:, 0:129],
                                                   func=AF.Copy, scale=dec[:, col:col + 1]),
                     r=[("Sst", c % 2), "dec"], w=[("Sbf", c % 2)])
                S.op("pe", lambda e: e.matmul(po[:, 0:129], QT[:, cs], Sb[:, 0:129], start=True,
                                              stop=False), r=[("QT", c // 4), ("Sbf", c % 2)],
                     w=[pok], sig=False)
            S.op("pe", lambda e: e.matmul(po[:, 0:129], P, Vaug[:, c, 0:129], start=(c == 0),
                                          stop=True),
                 r=[("P", c % 2), ("V", c // 4), "Vones"], w=[pok])
            S.op("act", lambda e: e.activation(out=HR[:, c, 0:129], in_=po[:, 0:129],
                                               func=AF.Copy), r=[pok], w=[("HR", c)])
        allHR = [("HR", c) for c in range(16)]
        den = HR[:, :, 128]
        S.op("dve", lambda e: e.tensor_tensor(out=ep[:, 0, :], in0=den, in1=uu3[:, :, h],
                                              op=ALU.mult), r=allHR + ["uu"], w=["ep0"])
        S.op("dve", lambda e: e.tensor_scalar(out=ep[:, 1, :], in0=ep[:, 0, :], scalar1=-1.0,
                                              scalar2=None, op0=ALU.mult), r=["ep0"], w=["ep1"])
        S.op("dve", lambda e: e.tensor_tensor(out=ep[:, 0, :], in0=ep[:, 0, :], in1=ep[:, 1, :],
                                              op=ALU.max), r=["ep0", "ep1"], w=["ep0"])
        S.op("dve", lambda e: e.tensor_tensor(out=ep[:, 0, :], in0=ep[:, 0, :], in1=enm3[:, :, h],
                                              op=ALU.max), r=["ep0", "enm"], w=["ep0"])
        S.op("dve", lambda e: e.reciprocal(out=ep[:, 0, :], in_=ep[:, 0, :]), r=["ep0"], w=["ep0"])
        S.op("dve", lambda e: e.tensor_tensor(out=ep[:, 2, :], in0=ep[:, 0, :], in1=uu3[:, :, h],
                                              op=ALU.mult), r=["ep0", "uu"], w=["ep2"])
        for c in range(16):
            S.op("act", lambda e: e.activation(out=junk, in_=HR[:, c, 0:128], func=AF.Square,
                                               accum_out=ep[:, 3, c:c + 1]),
                 r=[("HR", c)], w=["junk", ("ss", c)])
        S.op("dve", lambda e: e.tensor_tensor(out=ep[:, 4, :], in0=ep[:, 2, :], in1=ep[:, 2, :],
                                              op=ALU.mult), r=["ep2"], w=["ep4"])
        S.op("dve", lambda e: e.tensor_tensor(out=ep[:, 4, :], in0=ep[:, 4, :], in1=ep[:, 3, :],
                                              op=ALU.mult), r=["ep4"] + [("ss", c) for c in range(16)],
             w=["ep4"])
        S.op("act", lambda e: e.activation(out=ep[:, 4, :], in_=ep[:, 4, :], func=AF.Sqrt,
                                           scale=1.0 / 128, bias=C.epsc[:, 0:1]),
             r=["ep4", "epsc"], w=["ep4"])
        S.op("dve", lambda e: e.reciprocal(out=ep[:, 4, :], in_=ep[:, 4, :]), r=["ep4"], w=["ep4"])
        S.op("dve", lambda e: e.tensor_tensor(out=ep[:, 5, :], in0=ep[:, 4, :], in1=ep[:, 2, :],
                                              op=ALU.mult), r=["ep4", "ep2"], w=["ep5"])
        for c in range(16):
            S.op("dve", lambda e: e.scalar_tensor_tensor(
                out=Htok[:, c, :], in0=HR[:, c, 0:128], scalar=ep[:, 5, c:c + 1], in1=OG[:, c, :],
                op0=ALU.mult, op1=ALU.mult),
                r=[("HR", c), "ep5", ("OG", c // 4), ("KT", c // 4)], w=[("KT", c // 4)])
        for n4 in range(4):
            pb, pk = pbank(C)
            pbb = pb.bitcast(BF16)
            for i in range(4):
                n = n4 * 4 + i
                S.op("pe", lambda e: e.transpose(pbb[:, i * 128:(i + 1) * 128], Htok[:, n, :],
                                                 C.ident_bf[:]),
                     r=[("KT", n4), "ident_bf"], w=[pk], sig=(i == 3))
            S.op("act", lambda e: e.activation(out=QT[:, n4 * 512:(n4 + 1) * 512],
                                               in_=pbb[:, 0:512], func=AF.Copy),
                 r=[pk], w=[("QT", n4)])
        wt, wk = C.W.next(C.b_wout[h], D)
        for m in range(KC):
            for n in range(NTG):
                pb, pk = pbank(C)
                S.op("pe", lambda e: e.matmul(pb, wt[:, m * 128:(m + 1) * 128],
                                              QT[:, n * 512:(n + 1) * 512], start=True, stop=True),
                     r=[wk, ("QT", n)], w=[pk])
                S.op("dve", lambda e: e.tensor_tensor(
                    out=C.XT[:, m, n * 512:(n + 1) * 512], in0=pb,
                    in1=C.XT[:, m, n * 512:(n + 1) * 512], op=ALU.add),
                    r=[pk, ("XT", m, n)], w=[("XT", m, n)])


def mixer_stage(i):
    kind = i % 3
    def st(C):
        if kind == 2:
            gmlp(C, i)
        elif kind == 0:
            fox(C, i)
        else:
            mlstm(C, i)
    return st


def prep_shared(inp):
    sh = {}
    vecs = np.zeros((128, 80), np.float32)
    for i in range(DEPTH):
        vecs[:, 8 * i:8 * i + 8] = col_layout(inp["norm1_g"][i])
        vecs[:, 32 + 8 * i:32 + 8 * i + 8] = col_layout(inp["norm2_g"][i])
    vecs[:, 64:72] = col_layout(inp["final_g"])
    sh["vecs"] = vecs
    gu = np.empty((DEPTH, NF, 128, KC * 256), np.float32)
    dn = np.empty((DEPTH, KC, 128, NF * 128), np.float32)
    for i in range(DEPTH):
        W = inp["ffn_w_gu"][i]
        g = W[:, :DFF].reshape(KC, 128, NF, 128)
        u = W[:, DFF:].reshape(KC, 128, NF, 128)
        cat = np.concatenate([g, u], axis=3)
        gu[i] = cat.transpose(2, 1, 0, 3).reshape(NF, 128, KC * 256)
        Wd = inp["ffn_w_down"][i].reshape(NF, 128, KC, 128)
        dn[i] = Wd.transpose(2, 1, 0, 3).reshape(KC, 128, NF * 128)
    sh["ffn_gu"] = gu
    sh["ffn_dn"] = dn
    mvec = np.zeros((128, MV_NCOL), np.float32)
    mvec[:, MV_LNG:MV_LNG + 16] = col_layout(inp["c_ln_g"][0])
    for j in range(2):
        mvec[0:16, MV_ABF + j] = inp["a_b_f"][j]
    cwt = inp["b_conv_w"][0]
    for j in range(4):
        mvec[:, MV_CW + j * 16:MV_CW + (j + 1) * 16] = col_layout(cwt[j])
    mvec[:, MV_CB:MV_CB + 16] = col_layout(inp["b_conv_b"][0])
    mvec[0:8, MV_BI] = inp["b_b_i"][0]
    mvec[0:8, MV_BF] = inp["b_b_f"][0]
    sh["mvec"] = mvec
    Wb = inp["b_w_in"][0]
    bwin = np.empty((8, 128, KC * 512), np.float32)
    for h in range(8):
        cat = np.concatenate([Wb[:, h * 128:(h + 1) * 128], Wb[:, D + h * 128:D + (h + 1) * 128],
                              Wb[:, 2 * D + h * 128:2 * D + (h + 1) * 128],
                              Wb[:, 3 * D + h * 128:3 * D + (h + 1) * 128]], axis=1)
        bwin[h] = blockify_cols(cat, 512)[0]
    sh["b_win"] = bwin
    sh["b_wg"] = blockify_cols(Wb[:, 4 * D:4 * D + 16], 16)[0]
    sh["b_wout"] = np.ascontiguousarray(inp["b_w_out"][0].reshape(8, 128, D))
    sh["b_ng"] = np.ascontiguousarray(np.broadcast_to(inp["b_norm_g"][0][None, :], (128, D)))
    cst = np.zeros((128, 5, 128), np.float32)
    cst[:, 0, :] = np.eye(128, dtype=np.float32)
    cst[63, 1, :] = 1.0
    kk, qq = np.meshgrid(np.arange(128), np.arange(128), indexing="ij")
    cst[:, 2, :] = np.where(kk > qq, -30000.0, 0.0)
    cst[127, 3, :] = 1.0
    cst[:, 4, :] = (qq >= kk)
    sh["consts"] = cst.reshape(128, 640)
    awin = np.empty((2, 8, 128, KC * 384), np.float32)
    awf = np.empty((2, 128, KC * 16), np.float32)
    for j in range(2):
        W = inp["a_w_in"][j]
        for hp in range(8):
            cat = np.concatenate([W[:, hp * 128:(hp + 1) * 128], W[:, D + hp * 128:D + (hp + 1) * 128],
                                  W[:, 2 * D + hp * 128:2 * D + (hp + 1) * 128]], axis=1)
            awin[j, hp] = blockify_cols(cat, 384)[0]
        awf[j] = blockify_cols(W[:, 3 * D:3 * D + 16], 16)[0]
    sh["a_win"] = awin
    sh["a_wf"] = awf
    sh["a_wout"] = np.ascontiguousarray(inp["a_w_out"].reshape(2, 8, 128, D))
    sh["c_win"] = blockify_cols(inp["c_w_in"][0], 512)
    sh["c_wout"] = np.ascontiguousarray(
        inp["c_w_out"][0].reshape(16, 128, KC, 128).transpose(2, 1, 0, 3)).reshape(KC, 128, 16 * 128)
    sh["c_wsT"] = np.ascontiguousarray(inp["c_w_s"][0].transpose(2, 0, 1)).reshape(128, 1024)
    sh["c_lnb"] = np.ascontiguousarray(inp["c_ln_b"][0].reshape(1, C_W))
    sh["c_bs"] = np.ascontiguousarray(inp["c_b_s"][0].reshape(1, 1024))
    return sh


def stages_from_spec(spec):
    st = []
    for (kind, i) in spec:
        if kind == "ffn":
            st.append(ffn_stage(i))
        else:
            st.append(mixer_stage(i))
    return st


def run(inp, spec, ncores=8):
    inp = {k: np.asarray(v) for k, v in inp.items()}
    nc = build(stages_from_spec(spec))
    sh = prep_shared(inp)
    in_maps = []
    for b in range(ncores):
        m = dict(sh)
        m["xT"] = np.ascontiguousarray(inp["x"][b].T)
        in_maps.append(m)
    res = run_bass_kernel_spmd(nc, in_maps, core_ids=list(range(ncores)))
    return [np.ascontiguousarray(r["outT"].T) for r in res.results]


FULL_SPEC = []
for _i in range(DEPTH):
    FULL_SPEC += [("mix", _i), ("ffn", _i)]


def kernel(**inputs):
    outs = run(inputs, FULL_SPEC, ncores=8)
    return np.stack(outs, axis=0).astype(np.float32)
```
